# Optimizing a Trainium2 kernel written in Bass

```python
import jax
import jax.numpy as jnp
from jax import lax
import numpy as np

D_MODEL = 1024
BATCH = 8
SEQ = 8192
DEPTH = 1

ATT_HEADS = 4
ATT_HEAD_DIM = 128
ATT_WIDTH = ATT_HEADS * ATT_HEAD_DIM
IDX_HEADS = 8
IDX_DIM = 64
TOPK_MAX = 256
Q_BLOCK = 128
GLA_HEADS = 4
GLA_DK = 64
GLA_DV = 128
GLA_WIDTH = GLA_HEADS * GLA_DV
GLA_GATE_RANK = 16
GLA_TAU = 16.0
GLA_CHUNK = 64
D_MIX = ATT_WIDTH + GLA_WIDTH
D_FF = ((8 * D_MODEL + 2) // 3 + 255) // 256 * 256
N_MOD = 6
NORM_EPS = 1e-6
IN_SPLITS = (ATT_WIDTH, ATT_WIDTH, ATT_WIDTH,
             IDX_HEADS * IDX_DIM, IDX_DIM, IDX_HEADS,
             GLA_HEADS * GLA_DK, GLA_HEADS * GLA_DK,
             GLA_WIDTH, GLA_WIDTH, GLA_GATE_RANK)
D_IN = sum(IN_SPLITS)

kernel_name = 'hymba_dsa_gla_adaln_block'


def _rms_norm(x, g):
    xf = x.astype(jnp.float32)
    y = xf * lax.rsqrt(jnp.mean(xf * xf, axis=-1, keepdims=True) + NORM_EPS)
    return (y * g.astype(jnp.float32)).astype(x.dtype)


def _modulate(h, shift, scale):
    return h * (1 + scale[:, None, :]) + shift[:, None, :]


def _split_cols(p, sizes):
    offs = []
    acc = 0
    for s in sizes[:-1]:
        acc += s
        offs.append(acc)
    return jnp.split(p, offs, axis=-1)


def dsa_attention(q, k, v, q_idx, k_idx, w_idx):
    B, L, H, Dh = q.shape
    n_sel = min(TOPK_MAX, L // 4)
    n_blocks = L // Q_BLOCK
    att_scale = Dh ** -0.5
    idx_scale = (IDX_HEADS ** -0.5) * (IDX_DIM ** -0.5)
    key_pos = jnp.arange(L)
    k_idx_f = k_idx.astype(jnp.float32)

    def block(i):
        start = i * Q_BLOCK
        qb = lax.dynamic_slice_in_dim(q, start, Q_BLOCK, axis=1)
        qib = lax.dynamic_slice_in_dim(q_idx, start, Q_BLOCK, axis=1)
        wb = lax.dynamic_slice_in_dim(w_idx, start, Q_BLOCK, axis=1)
        q_pos = start + jnp.arange(Q_BLOCK)
        causal = key_pos[None, :] <= q_pos[:, None]
        s_idx = jnp.einsum('bthd,bsd->bths', qib.astype(jnp.float32), k_idx_f)
        score = jnp.einsum('bths,bth->bts', jax.nn.relu(s_idx),
                           wb.astype(jnp.float32) * idx_scale)
        score = jnp.where(causal[None], score, -jnp.inf)
        _, sel = lax.top_k(score, n_sel)
        valid = sel <= q_pos[None, :, None]
        kg = jax.vmap(lambda kk, ii: kk[ii])(k, sel)
        vg = jax.vmap(lambda vv, ii: vv[ii])(v, sel)
        logits = jnp.einsum('bthd,btjhd->bthj', qb, kg).astype(jnp.float32) * att_scale
        logits = jnp.where(valid[:, :, None, :], logits, -jnp.inf)
        p = jax.nn.softmax(logits, axis=-1).astype(v.dtype)
        return jnp.einsum('bthj,btjhd->bthd', p, vg)

    out = lax.map(block, jnp.arange(n_blocks))
    return out.transpose(1, 0, 2, 3, 4).reshape(B, L, H * Dh)


def gla_chunked(q, k, v, log_a):
    B, L, H, DK = q.shape
    DV = v.shape[-1]
    C = GLA_CHUNK
    NC = L // C

    def to_chunks(t):
        return t.astype(jnp.float32).reshape(B, NC, C, H, t.shape[-1]).transpose(1, 0, 3, 2, 4)

    qc_all = to_chunks(q) * (DK ** -0.5)
    kc_all, vc_all, gc_all = to_chunks(k), to_chunks(v), to_chunks(log_a)
    tril = jnp.tril(jnp.ones((C, C), dtype=bool))

    def step(S, inp):
        qc, kc, vc, gc = inp
        b = jnp.cumsum(gc, axis=2)
        diff = b[:, :, :, None, :] - b[:, :, None, :, :]
        decay = jnp.exp(jnp.where(tril[None, None, :, :, None], diff, -jnp.inf))
        A = jnp.einsum('bhid,bhjd,bhijd->bhij', qc, kc, decay)
        o = (jnp.einsum('bhij,bhjv->bhiv', A, vc)
             + jnp.einsum('bhid,bhdv->bhiv', qc * jnp.exp(b), S))
        b_last = b[:, :, -1:, :]
        S = (jnp.exp(b_last)[:, :, 0, :, None] * S
             + jnp.einsum('bhjd,bhjv->bhdv', kc * jnp.exp(b_last - b), vc))
        return S, o

    S0 = jnp.zeros((B, H, DK, DV), jnp.float32)
    _, o = lax.scan(step, S0, (qc_all, kc_all, vc_all, gc_all))
    return o.transpose(1, 0, 3, 2, 4).reshape(B, L, H, DV).astype(v.dtype)


def setup_inputs(seed: int = 0) -> dict:
    key = jax.random.key(seed)
    ks = jax.random.split(key, 16)

    def nrm(k, shape, scale):
        return jax.random.normal(k, shape, jnp.float32) * scale

    return {
        'x': nrm(ks[0], (BATCH, SEQ, D_MODEL), 1.0),
        'c': nrm(ks[1], (BATCH, D_MODEL), 1.0),
        'w_mod': nrm(ks[2], (DEPTH, D_MODEL, N_MOD * D_MODEL), D_MODEL ** -0.5),
        'b_mod': nrm(ks[3], (DEPTH, N_MOD * D_MODEL), 0.02),
        'norm1_g': 1.0 + nrm(ks[4], (DEPTH, D_MODEL), 0.02),
        'w_in': nrm(ks[5], (DEPTH, D_MODEL, D_IN), D_MODEL ** -0.5),
        'w_gate2': nrm(ks[6], (DEPTH, GLA_GATE_RANK, GLA_HEADS * GLA_DK), GLA_GATE_RANK ** -0.5),
        'b_gate2': nrm(ks[7], (DEPTH, GLA_HEADS * GLA_DK), 0.02),
        'att_out_g': 1.0 + nrm(ks[8], (DEPTH, ATT_WIDTH), 0.02),
        'gla_out_g': 1.0 + nrm(ks[9], (DEPTH, GLA_DV), 0.02),
        'w_out': nrm(ks[10], (DEPTH, D_MIX, D_MODEL), D_MIX ** -0.5),
        'norm2_g': 1.0 + nrm(ks[11], (DEPTH, D_MODEL), 0.02),
        'w_gate_up': nrm(ks[12], (DEPTH, D_MODEL, 2 * D_FF), D_MODEL ** -0.5),
        'w_down': nrm(ks[13], (DEPTH, D_FF, D_MODEL), D_FF ** -0.5),
        'final_g': 1.0 + nrm(ks[14], (D_MODEL,), 0.02),
    }


def reference(x, c, w_mod, b_mod, norm1_g, w_in, w_gate2, b_gate2, att_out_g,
              gla_out_g, w_out, norm2_g, w_gate_up, w_down, final_g):
    B, L, _ = x.shape
    for l in range(DEPTH):
        mod = jax.nn.silu(c) @ w_mod[l] + b_mod[l]
        sh1, sc1, g1, sh2, sc2, g2 = jnp.split(mod, N_MOD, axis=-1)

        h = _modulate(_rms_norm(x, norm1_g[l]), sh1, sc1)
        proj = h @ w_in[l]
        (q, k, v, q_idx, k_idx, w_idx,
         gq, gk, gv, go, g_lr) = _split_cols(proj, IN_SPLITS)

        att = dsa_attention(q.reshape(B, L, ATT_HEADS, ATT_HEAD_DIM),
                            k.reshape(B, L, ATT_HEADS, ATT_HEAD_DIM),
                            v.reshape(B, L, ATT_HEADS, ATT_HEAD_DIM),
                            q_idx.reshape(B, L, IDX_HEADS, IDX_DIM), k_idx, w_idx)
        att = _rms_norm(att, att_out_g[l])

        log_a = jax.nn.log_sigmoid((g_lr @ w_gate2[l] + b_gate2[l]).astype(jnp.float32)) / GLA_TAU
        gla = gla_chunked(gq.reshape(B, L, GLA_HEADS, GLA_DK),
                          gk.reshape(B, L, GLA_HEADS, GLA_DK),
                          gv.reshape(B, L, GLA_HEADS, GLA_DV),
                          log_a.reshape(B, L, GLA_HEADS, GLA_DK))
        gla = _rms_norm(gla, gla_out_g[l]).reshape(B, L, GLA_WIDTH) * jax.nn.silu(go)

        mix = jnp.concatenate([att, gla], axis=-1) @ w_out[l]
        x = x + g1[:, None, :] * mix

        h2 = _modulate(_rms_norm(x, norm2_g[l]), sh2, sc2)
        gate, up = jnp.split(h2 @ w_gate_up[l], 2, axis=-1)
        x = x + g2[:, None, :] * ((jax.nn.silu(gate) * up) @ w_down[l])
    return _rms_norm(x, final_g)
```

```python
from contextlib import ExitStack

import numpy as np
import ml_dtypes
import concourse.bass as bass
import concourse.mybir as mybir
from concourse.bass_utils import run_bass_kernel_spmd

F32 = mybir.dt.float32
BF16 = mybir.dt.bfloat16
U8 = mybir.dt.uint8
AF = mybir.ActivationFunctionType
ALU = mybir.AluOpType

D = 1024
DIN = 3672
DFF = 2816
NEG = -1.0e30
EPS = 1e-6
IDX_SCALE = (8 ** -0.5) * (64 ** -0.5)
ATT_SCALE = 128 ** -0.5
CUT = [0]
NIT = 16
TOPK = 256

C_Q, C_K, C_V, C_QI, C_KI, C_WI, C_GQ, C_GK, C_GV, C_GO, C_LR = (
    0, 512, 1024, 1536, 2048, 2112, 2120, 2376, 2632, 3144, 3656)


class Sched:
    def __init__(self, nc):
        self.nc = nc
        self.ops = []
        self.state = {}
        self.slot_last = {}

    def op(self, eng, fn, reads=(), writes=(), slot=None):
        i = len(self.ops)
        deps = set()
        for b in reads:
            st = self.state.get(b)
            if st and st[0] is not None:
                deps.add(st[0])
        for b in writes:
            st = self.state.get(b)
            if st:
                if st[0] is not None:
                    deps.add(st[0])
                deps.update(st[1])
        if slot is not None:
            p = self.slot_last.get(slot)
            if p is not None:
                deps.add(p)
            self.slot_last[slot] = i
        for b in writes:
            self.state[b] = [i, []]
        for b in reads:
            st = self.state.setdefault(b, [None, []])
            st[1].append(i)
        deps.discard(i)
        self.ops.append(dict(eng=eng, fn=fn, deps=deps, slot=slot))
        return i

    def emit(self, name):
        nc = self.nc
        ops = self.ops
        needed = set()
        for o in ops:
            nd = set()
            for d in o['deps']:
                od = ops[d]
                if (od['eng'] == 'pe' and o['eng'] == 'pe'
                        and od['slot'] is None and o['slot'] is None):
                    continue
                nd.add(d)
            o['deps'] = nd
            needed |= nd
        cnt = {}
        for i, o in enumerate(ops):
            if o['slot'] is not None:
                k = 'dma_' + o['slot']
                cnt[k] = cnt.get(k, 0) + 16
                o['tok'] = (k, cnt[k])
            elif i in needed:
                k = o['eng']
                cnt[k] = cnt.get(k, 0) + 1
                o['tok'] = (k, cnt[k])
            else:
                o['tok'] = None
        waited = {}
        per_eng = {}
        for o in ops:
            w = {}
            for d in o['deps']:
                k, v = ops[d]['tok']
                if v > w.get(k, 0):
                    w[k] = v
            e = o['eng']
            wl = []
            for k, v in sorted(w.items()):
                if v > waited.get((e, k), 0):
                    waited[(e, k)] = v
                    wl.append((k, v))
            o['waits'] = wl
            per_eng.setdefault(e, []).append(o)
        with ExitStack() as es:
            sems = {k: es.enter_context(nc.semaphore(name + '_' + k)) for k in sorted(cnt)}
            blk = es.enter_context(nc.Block())

            def body(engname):
                def f(e):
                    for o in per_eng.get(engname, []):
                        for k, v in o['waits']:
                            e.wait_ge(sems[k], v)
                        ins = o['fn'](e)
                        if o['tok'] is not None:
                            ins.then_inc(sems[o['tok'][0]], 16 if o['slot'] is not None else 1)
                    if engname == 'sp':
                        for k in sorted(cnt):
                            if k.startswith('dma_'):
                                e.wait_ge(sems[k], cnt[k])
                return f

            blk.tensor(body('pe'))
            blk.scalar(body('act'))
            blk.vector(body('dve'))
            blk.gpsimd(body('pool'))
            blk.sync(body('sp'))
        self.ops = []
        self.state = {}
        self.slot_last = {}


def MM(S, out, lhsT, rhs, start, stop, R, W):
    S.op('pe', lambda e: e.matmul(out, lhsT, rhs, start=start, stop=stop), R, W)


def TR(S, out, in_, ident, R, W):
    S.op('pe', lambda e: e.transpose(out, in_, ident), R, W)


def ACT(S, out, in_, func, R, W, bias=None, scale=None, accum=None):
    kw = {}
    if bias is not None:
        kw['bias'] = bias
    if scale is not None:
        kw['scale'] = scale
    if accum is not None:
        kw['accum_out'] = accum
    S.op('act', lambda e: e.activation(out, in_, func, **kw), R, W)


def TS(S, eng, out, in0, s1, s2, op0, op1, R, W, accum=None):
    if op1 is None:
        if accum is None:
            S.op(eng, lambda e: e.tensor_scalar(out, in0, s1, None, op0), R, W)
        else:
            S.op(eng, lambda e: e.tensor_scalar(out, in0, s1, None, op0, accum_out=accum), R, W)
    else:
        if accum is None:
            S.op(eng, lambda e: e.tensor_scalar(out, in0, s1, s2, op0, op1), R, W)
        else:
            S.op(eng, lambda e: e.tensor_scalar(out, in0, s1, s2, op0, op1, accum_out=accum), R, W)


def TT(S, eng, out, in0, in1, op, R, W):
    S.op(eng, lambda e: e.tensor_tensor(out, in0, in1, op), R, W)


def STT(S, out, in0, scalar, in1, op0, op1, R, W):
    S.op('dve', lambda e: e.scalar_tensor_tensor(out, in0, scalar, in1, op0, op1), R, W)


def CP(S, eng, out, in_, R, W):
    if eng == 'act':
        S.op('act', lambda e: e.activation(out, in_, AF.Copy), R, W)
    else:
        S.op(eng, lambda e: e.tensor_copy(out, in_), R, W)


def DMA(S, eng, out, in_, R, W, slot):
    S.op(eng, lambda e: e.dma_start(out=out, in_=in_), R, W, slot=slot)


def build_nc(L, debug=False, phases=(0, 1, 2, 3)):
    NT = L // 128
    NS1 = L // 512
    nc = bass.Bass("TRN2", target_bir_lowering=False)

    def din(name, shape, dt=F32):
        return nc.dram_tensor(name, list(shape), dt, kind="ExternalInput").ap()

    x = din("x", [L, D])
    c_pm = din("c_pm", [128, 8])
    w_mod = din("w_mod", [D, 6 * D])
    b_mod_pm = din("b_mod_pm", [128, 48])
    n1g_pm = din("n1g_pm", [128, 8])
    w_in = din("w_in", [D, DIN])
    w_gate2 = din("w_gate2", [16, 256])
    b_gate2_pm = din("b_gate2_pm", [128, 2])
    mixg_pm = din("mixg_pm", [128, 8])
    w_out = din("w_out", [D, D])
    n2g_pm = din("n2g_pm", [128, 8])
    w_gate_up = din("w_gate_up", [D, 2 * DFF])
    w_down = din("w_down", [DFF, D])
    fing_bc = din("fing_bc", [128, D])
    cf32 = din("cf32", [128, 768])
    cbf = din("cbf", [128, 1152], BF16)

    okind = "ExternalOutput"
    skind = "ExternalOutput" if debug else "Internal"
    out = nc.dram_tensor("out", [L, D], F32, kind=okind).ap()

    def scr(name, shape, dt):
        return nc.dram_tensor(name, list(shape), dt, kind=skind).ap()

    qT_s = scr("qT_s", [4, 128, L], BF16)
    kT_s = scr("kT_s", [4, 128, L], BF16)
    v_s = scr("v_s", [L, 512], BF16)
    qiT_s = scr("qiT_s", [4, 128, L], BF16)
    kiT_s = scr("kiT_s", [64, L], BF16)
    wab_s = scr("wab_s", [L, 16], F32)
    mix_s = scr("mix_s", [L, 1024], BF16)

    S = Sched(nc)

    with ExitStack() as top:
        def sb(name, shape, dt):
            return top.enter_context(nc.sbuf_tensor(name, list(shape), dt))

        modT = sb("modT", [128, 48], F32)
        AB = sb("AB", [128, 32], F32)
        identb = sb("identb", [128, 128], BF16)
        tri4b = sb("tri4b", [128, 512], BF16)
        identf = sb("identf", [128, 128], F32)
        cmaskf = sb("cmaskf", [128, 128], F32)
        onesf = sb("onesf", [128, 128], F32)

        with ExitStack() as ph:
            def sbp(name, shape, dt):
                return ph.enter_context(nc.sbuf_tensor(name, list(shape), dt))

            def psp(name, shape, dt):
                return ph.enter_context(nc.psum_tensor(name, list(shape), dt))

            c_sb = sbp("c_sb", [128, 8], F32)
            sc_sb = sbp("sc_sb", [128, 8], F32)
            tmp8 = sbp("tmp8", [128, 8], F32)
            bm_sb = sbp("bm_sb", [128, 48], F32)
            g_sb = sbp("g_sb", [128, 16], F32)
            wm = [sbp("wm%d" % i, [128, 6 * D], F32) for i in range(2)]
            ps0 = psp("ps0", [128, 48], F32)

            DMA(S, 'sp', c_sb[:], c_pm[:, :], [], ['c_sb'], 'a')
            DMA(S, 'sp', bm_sb[:], b_mod_pm[:, :], [], ['bm_sb'], 'b')
            DMA(S, 'sp', g_sb[:, 0:8], n1g_pm[:, :], [], ['g_sb'], 'c')
            DMA(S, 'sp', g_sb[:, 8:16], n2g_pm[:, :], [], ['g_sb2'], 'd')
            DMA(S, 'pool', identb[:], cbf[:, 0:128], [], ['identb'], 'e')
            DMA(S, 'pool', tri4b[:], cbf[:, 128:640], [], ['tri4b'], 'f')
            DMA(S, 'pool', identf[:], cf32[:, 0:128], [], ['identf'], 'g')
            DMA(S, 'pool', cmaskf[:], cf32[:, 128:256], [], ['cmaskf'], 'h')
            DMA(S, 'pool', onesf[:], cf32[:, 256:384], [], ['onesf'], 'i')
            ACT(S, tmp8[:], c_sb[:], AF.Exp, ['c_sb'], ['tmp8'], scale=-1.0)
            TS(S, 'dve', tmp8[:], tmp8[:], 1.0, None, ALU.add, None, ['tmp8'], ['tmp8'])
            S.op('dve', lambda e: e.reciprocal(tmp8[:], tmp8[:]), ['tmp8'], ['tmp8'])
            TT(S, 'dve', sc_sb[:], c_sb[:], tmp8[:], ALU.mult, ['c_sb', 'tmp8'], ['sc_sb'])
            for k in range(8):
                b = k % 2
                DMA(S, 'sp' if b == 0 else 'pool', wm[b][:], w_mod[k * 128:(k + 1) * 128, :],
                    [], ['wm%d' % b], 'wm%d' % b)
                for j in range(48):
                    MM(S, ps0[:, j:j + 1], wm[b][:, j * 128:(j + 1) * 128], sc_sb[:, k:k + 1],
                       True, True, ['wm%d' % b, 'sc_sb'], ['ps0'])
                TT(S, 'dve', modT[:], ps0[:], bm_sb[:] if k == 0 else modT[:], ALU.add,
                   ['ps0', 'bm_sb', 'modT'], ['modT'])
            STT(S, AB[:, 0:8], modT[:, 8:16], 1.0, g_sb[:, 0:8], ALU.add, ALU.mult,
                ['modT', 'g_sb'], ['AB'])
            CP(S, 'dve', AB[:, 8:16], modT[:, 0:8], ['modT'], ['AB1'])
            STT(S, AB[:, 16:24], modT[:, 32:40], 1.0, g_sb[:, 8:16], ALU.add, ALU.mult,
                ['modT', 'g_sb2'], ['AB2'])
            CP(S, 'dve', AB[:, 24:32], modT[:, 24:32], ['modT'], ['AB3'])
            S.emit("p0")

        if 1 in phases:
            phase1(nc, S, L, x, w_in, w_gate2, b_gate2_pm, AB, identb, tri4b, identf, onesf,
                   qT_s, kT_s, v_s, qiT_s, kiT_s, wab_s, mix_s)
        if 2 in phases:
            phase2(nc, S, L, cbf, cmaskf, qT_s, kT_s, v_s, qiT_s, kiT_s, wab_s, mix_s)
        if 3 in phases:
            phase3(nc, S, L, x, w_out, w_gate_up, w_down, mixg_pm, fing_bc, modT, AB, identb, identf,
                   onesf, mix_s, out)
    return nc


def phase1(nc, S, L, x, w_in, w_gate2, b_gate2_pm, AB, identb, tri4b, identf, onesf,
           qT_s, kT_s, v_s, qiT_s, kiT_s, wab_s, mix_s):
    NS1 = L // 512
    with ExitStack() as ph:
        def sbp(name, shape, dt):
            return ph.enter_context(nc.sbuf_tensor(name, list(shape), dt))

        def psp(name, shape, dt):
            return ph.enter_context(nc.psum_tensor(name, list(shape), dt))

        wbf = sbp("wbf", [128, 8, DIN], BF16)
        stg = [sbp("stg%d" % i, [128, DIN], F32) for i in range(2)]
        wg2 = sbp("wg2", [16, 256], F32)
        bg2 = sbp("bg2", [128, 2], F32)
        nbg2 = sbp("nbg2", [128, 2], F32)
        x_sb = sbp("x_sb", [128, 4, D], F32)
        junk = sbp("junk", [128, D], BF16)
        ss = sbp("ss", [128, 4], F32)
        rstd = sbp("rstd", [128, 4], F32)
        xn = [sbp("xn%d" % i, [128, D], BF16) for i in range(2)]
        hT = sbp("hT", [128, 8, 512], BF16)
        fmo = [sbp("fmo%d" % i, [128, 512], BF16) for i in range(4)]
        gqT = sbp("gqT", [128, 2, 512], F32)
        gkT = sbp("gkT", [128, 2, 512], F32)
        lrT = sbp("lrT", [16, 512], F32)
        lneg = sbp("lneg", [128, 2, 512], F32)
        ccum = sbp("ccum", [128, 2, 512], F32)
        v_tm = sbp("v_tm", [128, 4, 512], BF16)
        gv_tm = sbp("gv_tm", [128, 4, 512], BF16)
        sgo = sbp("sgo", [128, 4, 512], F32)
        wab = sbp("wab", [128, 4, 16], F32)
        Eq = sbp("Eq", [128, 2, 128], F32)
        Ek = sbp("Ek", [128, 2, 128], F32)
        Ed = sbp("Ed", [128, 2, 128], F32)
        nb = sbp("nb", [128, 2], F32)
        EL = sbp("EL", [128, 2], F32)
        qeT = sbp("qeT", [128, 2, 128], BF16)
        keM = sbp("keM", [128, 4, 128], BF16)
        kdT = sbp("kdT", [128, 2, 128], BF16)
        kd = sbp("kd", [128, 256], BF16)
        AT = sbp("AT", [128, 4, 128], BF16)
        Sf = sbp("Sf", [128, 2, 128], F32)
        Sb = sbp("Sb", [128, 4, 128], BF16)
        oss = sbp("oss", [128, 4], F32)
        orstd = sbp("orstd", [128, 4], F32)
        ojunk = sbp("ojunk", [128, 128], F32)
        glao = [sbp("glao%d" % i, [128, 512], BF16) for i in range(2)]
        ones1 = sbp("ones1", [128, 128], F32)

        pT = [psp("pT%d" % i, [128, D], BF16) for i in range(2)]
        pF = [psp("pF%d" % i, [128, 512], F32) for i in range(2)]
        pM = [psp("pM%d" % i, [128, 512], F32) for i in range(4)]

        for k in range(8):
            b = k % 2
            DMA(S, 'sp' if b == 0 else 'pool', stg[b][:], w_in[k * 128:(k + 1) * 128, :],
                [], ['stg%d' % b], 'stg%d' % b)
            CP(S, 'dve', wbf[:, k, 0:1836], stg[b][:, 0:1836], ['stg%d' % b], ['wbf%d' % k])
            CP(S, 'act', wbf[:, k, 1836:DIN], stg[b][:, 1836:DIN], ['stg%d' % b], ['wbf%d_' % k])
        WB = ['wbf%d' % k for k in range(8)] + ['wbf%d_' % k for k in range(8)]
        DMA(S, 'sp', wg2[:], w_gate2[:, :], [], ['wg2'], 'wg2')
        DMA(S, 'sp', bg2[:], b_gate2_pm[:, :], [], ['bg2'], 'bg2')
        TS(S, 'dve', nbg2[:], bg2[:], -1.0, None, ALU.mult, None, ['bg2'], ['nbg2'])
        S.op('dve', lambda e: e.memset(Sf[:], 0.0), [], ['Sf'])
        S.op('dve', lambda e: e.memset(Sb[:], 0.0), [], ['Sb'])
        S.op('dve', lambda e: e.memset(keM[:], 0.0), [], ['keM'])
        S.op('dve', lambda e: e.memset(ones1[:], 1.0), [], ['ones1'])

        fmo_i = [0]
        st_i = [0]

        def store_slot():
            st_i[0] += 1
            return 'st%d' % (st_i[0] % 8)

        for st in range(NS1):
            t0 = st * 512
            DMA(S, 'sp', x_sb[:], x[t0:t0 + 512, :].rearrange("(j p) d -> p j d", p=128),
                [], ['x_sb'], 'x')
            for j in range(4):
                ACT(S, junk[:], x_sb[:, j, :], AF.Square, ['x_sb'], ['junk', 'ss'],
                    accum=ss[:, j:j + 1])
            ACT(S, rstd[:], ss[:], AF.Ln, ['ss'], ['rstd'], bias=EPS, scale=1.0 / D)
            ACT(S, rstd[:], rstd[:], AF.Exp, ['rstd'], ['rstd'], scale=-0.5)
            for j in range(4):
                b = j % 2
                ACT(S, xn[b][:], x_sb[:, j, :], AF.Copy, ['x_sb', 'rstd'], ['xn%d' % b],
                    scale=rstd[:, j:j + 1])
                for k in range(8):
                    TR(S, pT[b][:, k * 128:(k + 1) * 128], xn[b][:, k * 128:(k + 1) * 128],
                       identb[:], ['xn%d' % b, 'identb'], ['pT%d' % b])
                for k in range(8):
                    eng = 'dve' if k % 2 == 0 else 'pool'
                    eng = 'dve'
                    TS(S, eng, hT[:, k, j * 128:(j + 1) * 128], pT[b][:, k * 128:(k + 1) * 128],
                       AB[:, k:k + 1], AB[:, 8 + k:9 + k], ALU.mult, ALU.add,
                       ['pT%d' % b, 'AB', 'AB1'], ['hT'])

            if CUT[0] == 1:
                S.emit('p1')
                return
            def fm(col, m, pidx):
                p = pF[pidx]
                for k in range(8):
                    MM(S, p[0:m, :], wbf[:, k, col:col + m], hT[:, k, :], k == 0, k == 7,
                       ['hT'] + WB, ['pF%d' % pidx])
                return p

            pi = 0
            for (col0, dst) in ((C_Q, qT_s), (C_K, kT_s), (C_QI, qiT_s)):
                for cch in range(4):
                    p = fm(col0 + cch * 128, 128, pi)
                    f = fmo_i[0] % 4
                    fmo_i[0] += 1
                    if pi == 0:
                        CP(S, 'dve', fmo[f][:], p[:, :], ['pF%d' % pi], ['fmo%d' % f])
                    else:
                        CP(S, 'act', fmo[f][:], p[:, :], ['pF%d' % pi], ['fmo%d' % f])
                    DMA(S, 'pool', dst[cch, :, t0:t0 + 512], fmo[f][:], ['fmo%d' % f], [],
                        store_slot())
                    pi ^= 1
            p = fm(C_KI, 64, pi)
            f = fmo_i[0] % 4
            fmo_i[0] += 1
            CP(S, 'dve', fmo[f][0:64, :], p[0:64, :], ['pF%d' % pi], ['fmo%d' % f])
            DMA(S, 'pool', kiT_s[:, t0:t0 + 512], fmo[f][0:64, :], ['fmo%d' % f], [], store_slot())
            pi ^= 1
            for cch in range(2):
                p = fm(C_GQ + cch * 128, 128, pi)
                CP(S, 'act', gqT[:, cch, :], p[:, :], ['pF%d' % pi], ['gqT'])
                pi ^= 1
                p = fm(C_GK + cch * 128, 128, pi)
                CP(S, 'dve', gkT[:, cch, :], p[:, :], ['pF%d' % pi], ['gkT'])
                pi ^= 1
            p = fm(C_LR, 16, pi)
            CP(S, 'dve', lrT[:], p[0:16, :], ['pF%d' % pi], ['lrT'])
            pi ^= 1
            if CUT[0] == 2:
                S.emit('p1')
                return
            for cch in range(2):
                MM(S, pF[pi][:, :], wg2[:, cch * 128:(cch + 1) * 128], lrT[:], True, True,
                   ['wg2', 'lrT'], ['pF%d' % pi])
                ACT(S, lneg[:, cch, :], pF[pi][:, :], AF.Exp, ['pF%d' % pi, 'nbg2'], ['lneg'],
                    bias=nbg2[:, cch:cch + 1], scale=-1.0)
                pi ^= 1
            ACT(S, lneg[:], lneg[:], AF.Ln, ['lneg'], ['lneg'], bias=1.0)
            for cch in range(2):
                for j in range(4):
                    S.op('dve', lambda e, cch=cch, j=j: e.tensor_tensor_scan(
                        ccum[:, cch, j * 128:(j + 1) * 128], ones1[:, :],
                        lneg[:, cch, j * 128:(j + 1) * 128], 0.0, ALU.mult, ALU.add),
                         ['lneg', 'ones1'], ['ccum'])

            if CUT[0] == 3:
                S.emit('p1')
                return
            for j in range(4):
                r0 = t0 + j * 128
                for gi, (col, n) in enumerate(((C_V, 512), (C_GV, 512), (C_GO, 512), (C_WI, 8))):
                    for k in range(8):
                        MM(S, pM[gi][:, 0:n], hT[:, k, j * 128:(j + 1) * 128],
                           wbf[:, k, col:col + n], k == 0, k == 7, ['hT'] + WB, ['pM%d' % gi])
                CP(S, 'act', v_tm[:, j, :], pM[0][:, :], ['pM0'], ['v_tm'])
                CP(S, 'dve', gv_tm[:, j, :], pM[1][:, :], ['pM1'], ['gv_tm'])
                ACT(S, sgo[:, j, :], pM[2][:, :], AF.Exp, ['pM2'], ['sgo'], scale=-1.0)
                TS(S, 'dve', sgo[:, j, :], sgo[:, j, :], 1.0, None, ALU.add, None, ['sgo'], ['sgo'])
                S.op('dve', lambda e, j=j: e.reciprocal(sgo[:, j, :], sgo[:, j, :]), ['sgo'], ['sgo'])
                TT(S, 'dve', sgo[:, j, :], sgo[:, j, :], pM[2][:, :], ALU.mult, ['sgo', 'pM2'], ['sgo'])
                ACT(S, wab[:, j, 0:8], pM[3][:, 0:8], AF.Abs, ['pM3'], ['wab'], scale=IDX_SCALE)
                ACT(S, wab[:, j, 8:16], pM[3][:, 0:8], AF.Sign, ['pM3'], ['wab'])
            DMA(S, 'pool', v_s[t0:t0 + 512, :].rearrange("(j p) d -> p j d", p=128), v_tm[:],
                ['v_tm'], [], store_slot())
            DMA(S, 'pool', wab_s[t0:t0 + 512, :].rearrange("(j p) d -> p j d", p=128), wab[:],
                ['wab'], [], store_slot())

            if CUT[0] == 4:
                S.emit('p1')
                return
            for j in range(4):
                r0 = t0 + j * 128
                cs = slice(j * 128, (j + 1) * 128)
                ACT(S, Eq[:], ccum[:, :, cs], AF.Exp, ['ccum'], ['Eq'], scale=-1.0 / 16)
                ACT(S, Ek[:], ccum[:, :, cs], AF.Exp, ['ccum'], ['Ek'], scale=1.0 / 16)
                for cch in range(2):
                    TS(S, 'dve', nb[:, cch:cch + 1], ccum[:, cch, j * 128 + 127:j * 128 + 128],
                       -1.0 / 16, None, ALU.mult, None, ['ccum'], ['nb'])
                for cch in range(2):
                    ACT(S, Ed[:, cch, :], ccum[:, cch, cs], AF.Exp, ['ccum', 'nb'], ['Ed'],
                        bias=nb[:, cch:cch + 1], scale=1.0 / 16)
                ACT(S, EL[:], nb[:], AF.Exp, ['nb'], ['EL'])
                if CUT[0] == 5:
                    S.emit('p1')
                    return
                STT(S, qeT[:], gqT[:, :, cs], 0.125, Eq[:], ALU.mult, ALU.mult,
                    ['gqT', 'Eq'], ['qeT'])
                for h in range(4):
                    ps_ = slice((h % 2) * 64, (h % 2) * 64 + 64)
                    TT(S, 'dve', keM[ps_, h, :], gkT[ps_, h // 2, cs], Ek[ps_, h // 2, :], ALU.mult,
                       ['gkT', 'Ek'], ['keM'])
                TT(S, 'dve', kdT[:], gkT[:, :, cs], Ed[:], ALU.mult, ['gkT', 'Ed'], ['kdT'])
                if CUT[0] == 6:
                    S.emit('p1')
                    return
                for cch in range(2):
                    TR(S, pT[0][:, cch * 128:(cch + 1) * 128], kdT[:, cch, :], identb[:],
                       ['kdT', 'identb'], ['pT0'])
                CP(S, 'act', kd[:], pT[0][:, 0:256], ['pT0'], ['kd'])
                if CUT[0] == 7:
                    S.emit('p1')
                    return
                for h in range(4):
                    ps_ = slice((h % 2) * 64, (h % 2) * 64 + 64)
                    MM(S, pF[0][:, h * 128:(h + 1) * 128], keM[:, h, :], qeT[:, h // 2, :],
                       True, True, ['keM', 'qeT'], ['pF0'])
                TT(S, 'dve', AT[:].rearrange("p h i -> p (h i)"), pF[0][:, :], tri4b[:], ALU.mult,
                   ['pF0', 'tri4b'], ['AT'])
                if CUT[0] == 8:
                    S.emit('p1')
                    return
                for h in range(4):
                    ps_ = slice((h % 2) * 64, (h % 2) * 64 + 64)
                    MM(S, pF[1][:, h * 128:(h + 1) * 128], AT[:, h, :],
                       gv_tm[:, j, h * 128:(h + 1) * 128], True, False, ['AT', 'gv_tm'], ['pF1'])
                    MM(S, pF[1][:, h * 128:(h + 1) * 128], qeT[:, h // 2, :], Sb[:, h, :],
                       False, True, ['qeT', 'Sb'], ['pF1'])
                if CUT[0] == 9:
                    S.emit('p1')
                    return
                for cch in range(2):
                    MM(S, pM[0][:, cch * 256:(cch + 1) * 256], kd[:, cch * 128:(cch + 1) * 128],
                       gv_tm[:, j, cch * 256:(cch + 1) * 256], True, True, ['kd', 'gv_tm'], ['pM0'])
                for h in range(4):
                    cch, hh = h // 2, h % 2
                    ps_ = slice(hh * 64, hh * 64 + 64)
                    STT(S, Sf[ps_, cch, :], Sf[ps_, cch, :], EL[ps_, cch:cch + 1],
                        pM[0][ps_, cch * 256 + hh * 128:cch * 256 + hh * 128 + 128],
                        ALU.mult, ALU.add, ['Sf', 'EL', 'pM0'], ['Sf'])
                for h in range(4):
                    ps_ = slice((h % 2) * 64, (h % 2) * 64 + 64)
                    CP(S, 'dve', Sb[ps_, h, :], Sf[ps_, h // 2, :], ['Sf'], ['Sb'])
                if CUT[0] == 10:
                    S.emit('p1')
                    return
                for h in range(4):
                    ACT(S, ojunk[:], pF[1][:, h * 128:(h + 1) * 128], AF.Square, ['pF1'],
                        ['ojunk', 'oss'], accum=oss[:, h:h + 1])
                ACT(S, orstd[:], oss[:], AF.Ln, ['oss'], ['orstd'], bias=EPS, scale=1.0 / 128)
                ACT(S, orstd[:], orstd[:], AF.Exp, ['orstd'], ['orstd'], scale=-0.5)
                g = j % 2
                for h in range(4):
                    STT(S, glao[g][:, h * 128:(h + 1) * 128], pF[1][:, h * 128:(h + 1) * 128],
                        orstd[:, h:h + 1], sgo[:, j, h * 128:(h + 1) * 128], ALU.mult, ALU.mult,
                        ['pF1', 'orstd', 'sgo'], ['glao%d' % g])
                DMA(S, 'pool', mix_s[r0:r0 + 128, 512:1024], glao[g][:], ['glao%d' % g], [],
                    store_slot())
        S.emit("p1")


def make_consts():
    ident = np.eye(128, dtype=np.float32)
    jj = np.arange(128)[:, None]
    ii = np.arange(128)[None, :]
    tri = (jj <= ii).astype(np.float32)
    cmask = np.where(ii > jj, NEG, 0.0).astype(np.float32)
    ones = np.ones((128, 128), np.float32)
    cf32 = np.concatenate([ident, cmask, ones, np.zeros((128, 384), np.float32)], axis=1)
    negi = -30000.0 * ident
    cbf = np.concatenate([ident, tri, tri, tri, tri, negi, negi, negi, negi], axis=1).astype(ml_dtypes.bfloat16)
    return np.ascontiguousarray(cf32), np.ascontiguousarray(cbf)


def pm(v, n):
    return np.ascontiguousarray(np.asarray(v, np.float32).reshape(n, 128).T)


def prep_core_inputs(inp, b, L):
    cf32, cbf = make_consts()
    f = lambda a: np.ascontiguousarray(np.asarray(a, np.float32))
    mixg = np.concatenate([np.asarray(inp['att_out_g'][0], np.float32),
                           np.tile(np.asarray(inp['gla_out_g'][0], np.float32), 4)])
    return {
        "x": f(inp['x'][b, :L]),
        "c_pm": pm(inp['c'][b], 8),
        "w_mod": f(inp['w_mod'][0]),
        "b_mod_pm": pm(inp['b_mod'][0], 48),
        "n1g_pm": pm(inp['norm1_g'][0], 8),
        "w_in": f(inp['w_in'][0]),
        "w_gate2": f(inp['w_gate2'][0]),
        "b_gate2_pm": pm(inp['b_gate2'][0], 2),
        "mixg_pm": pm(mixg, 8),
        "w_out": f(inp['w_out'][0]),
        "n2g_pm": pm(inp['norm2_g'][0], 8),
        "w_gate_up": f(inp['w_gate_up'][0]),
        "w_down": f(inp['w_down'][0]),
        "fing_bc": np.ascontiguousarray(np.broadcast_to(
            np.asarray(inp['final_g'], np.float32)[None, :], (128, D))),
        "cf32": cf32,
        "cbf": cbf,
    }


_NC_CACHE = {}


def kernel(**inputs):
    L = inputs['x'].shape[1]
    B = inputs['x'].shape[0]
    if L not in _NC_CACHE:
        _NC_CACHE[L] = build_nc(L)
    nc = _NC_CACHE[L]
    in_maps = [prep_core_inputs(inputs, b, L) for b in range(B)]
    res = run_bass_kernel_spmd(nc, in_maps, core_ids=list(range(B)))
    return np.stack([np.asarray(r["out"], np.float32) for r in res.results], axis=0)


def phase2(nc, S, L, cbf, cmaskf, qT_s, kT_s, v_s, qiT_s, kiT_s, wab_s, mix_s):
    NT = L // 128
    with ExitStack() as ph:
        def sbp(name, shape, dt):
            return ph.enter_context(nc.sbuf_tensor(name, list(shape), dt))

        def psp(name, shape, dt):
            return ph.enter_context(nc.psum_tensor(name, list(shape), dt))

        KT = sbp("KT", [128, 4, L], BF16)
        negI4 = sbp("negI4", [128, 512], BF16)
        Vp = sbp("Vp", [128, NT, 4, 132], BF16)
        kiT2 = sbp("kiT2", [128, L], BF16)
        score = sbp("score", [128, L], F32)
        cjunk = sbp("cjunk", [128, 4096], U8)
        ajk = sbp("ajk", [128, 4608], U8)
        hwk = sbp("hwk", [128, NIT + 1], F32)
        p2k = sbp("p2k", [128, NIT + 1], F32)
        sacc = sbp("sacc", [128, 1], F32)
        uu = sbp("uu", [128, 1], F32)
        maskb = [sbp("maskb%d" % i, [128, 512], BF16) for i in range(2)]
        cnt4 = sbp("cnt4", [128, 4], F32)
        qT_t = sbp("qT_t", [128, 4, 128], BF16)
        qiT_m = sbp("qiT_m", [128, 8, 128], BF16)
        wab_t = sbp("wab_t", [128, 16], F32)
        Rb = [sbp("Rb%d" % i, [128, 512], F32) for i in range(2)]
        PT = [sbp("PT%d" % i, [128, 4, 128], BF16) for i in range(3)]
        lo = sbp("lo", [128, 1], F32)
        hw = sbp("hw", [128, 1], F32)
        mid = sbp("mid", [128, 1], F32)
        cnt = sbp("cnt", [128, 1], F32)
        stp = sbp("stp", [128, 1], F32)
        mx = sbp("mx", [128, 1], F32)
        att_o = sbp("att_o", [128, 512], F32)
        rs = sbp("rs", [128, 4], F32)
        ass = sbp("ass", [128, 1], F32)
        arstd = sbp("arstd", [128, 1], F32)
        att_b = [sbp("att_b%d" % i, [128, 512], BF16) for i in range(2)]

        pI = [psp("pI%d" % i, [128, 512], F32) for i in range(2)]
        pL = [psp("pL%d" % i, [128, 4, 128], F32) for i in range(2)]
        pO = [psp("pO%d" % i, [128, 2, 132], F32) for i in range(2)]

        DMA(S, 'pool', negI4[:], cbf[:, 640:1152], [], ['negI4'], 'negI4')
        DMA(S, 'sp', KT[:], kT_s.rearrange("h d t -> d h t"), [], ['KT'], 'KT')
        DMA(S, 'pool', kiT2[0:64, :], kiT_s[:, :], [], ['kiT2'], 'ki0')
        DMA(S, 'pool', kiT2[64:128, :], kiT_s[:, :], [], ['kiT2b'], 'ki1')
        S.op('dve', lambda e: e.memset(Vp[:, :, :, 128:129], 1.0), [], ['Vp1'])
        S.op('dve', lambda e: e.memset(qiT_m[:], 0.0), [], ['qiT_m0', 'qiT_m1'])
        for kk in range(NIT + 1):
            S.op('dve', lambda e, kk=kk: e.memset(p2k[:, kk:kk + 1], 0.5 ** (kk + 1)), [], ['p2k'])
        for c0 in range(0, NT, 8):
            c1 = min(NT, c0 + 8)
            for h in range(4):
                DMA(S, 'sp' if h % 2 == 0 else 'pool', Vp[:, c0:c1, h, 0:128],
                    v_s[c0 * 128:c1 * 128, h * 128:(h + 1) * 128].rearrange("(c p) d -> p c d", p=128),
                    [], ['Vp_%d_%d' % (c0, h)], 'Vp%d' % h)
        VPK = ['Vp1'] + ['Vp_%d_%d' % (c0, h) for c0 in range(0, NT, 8) for h in range(4)]

        ri = [0]
        ii = [0]
        li = [0]
        mk = [0]

        def SK(i):
            return ['score%d' % kb for kb in range((i + 4) // 4)]

        def load_I(i):
            r0 = i * 128
            DMA(S, 'sp', qiT_m[0:64, 0:8:2, :], qiT_s[:, 0:64, r0:r0 + 128].rearrange("h d t -> d h t"), [],
                ['qiT_m0'], 'qi0')
            DMA(S, 'sp', qiT_m[64:128, 1:8:2, :], qiT_s[:, 64:128, r0:r0 + 128].rearrange("h d t -> d h t"), [],
                ['qiT_m1'], 'qi1')
            DMA(S, 'sp', wab_t[:], wab_s[r0:r0 + 128, :], [], ['wab_t'], 'wab')

        def load_A(i):
            r0 = i * 128
            DMA(S, 'sp', qT_t[:], qT_s[:, :, r0:r0 + 128].rearrange("h d t -> d h t"), [], ['qT_t'], 'q')

        def I_block(i, kb):
            n = (i + 1) * 128
            wkb = min(512, n - kb * 512)
            blk = slice(kb * 512, kb * 512 + wkb)
            sk = 'score%d' % kb
            for h in range(8):
                p = ii[0] % 2
                ii[0] += 1
                r = ri[0] % 2
                ri[0] += 1
                MM(S, pI[p][:, 0:wkb], qiT_m[:, h, :], kiT2[:, blk], True, True,
                   ['qiT_m0', 'qiT_m1', 'kiT2', 'kiT2b'], ['pI%d' % p])
                ACT(S, Rb[r][:, 0:wkb], pI[p][:, 0:wkb], AF.Relu, ['pI%d' % p, 'wab_t'],
                    ['Rb%d' % r], scale=wab_t[:, h:h + 1])
                if h == 0:
                    TS(S, 'dve', score[:, blk], Rb[r][:, 0:wkb], wab_t[:, 8:9], None, ALU.mult,
                       None, ['Rb%d' % r, 'wab_t'], [sk])
                else:
                    STT(S, score[:, blk], Rb[r][:, 0:wkb], wab_t[:, 8 + h:9 + h], score[:, blk],
                        ALU.mult, ALU.add, ['Rb%d' % r, 'wab_t', sk], [sk])
                yield

        def B_stage(i):
            n = (i + 1) * 128
            r0 = i * 128
            sk = SK(i)
            dk = 'score%d' % (i // 4)
            if i >= 2:
                S.op('dve', lambda e, n=n: e.tensor_reduce(mx[:], score[:, 0:n], mybir.AxisListType.X,
                                                          ALU.max), sk, ['mx'])
                S.op('dve', lambda e, n=n: e.tensor_reduce(lo[:], score[:, 0:n], mybir.AxisListType.X,
                                                          ALU.min), sk, ['lo'])
                TS(S, 'dve', lo[:], lo[:], -1.0, None, ALU.add, None, ['lo'], ['lo'])
                TS(S, 'dve', hw[:], mx[:], lo[:, 0:1], None, ALU.subtract, None, ['mx', 'lo'], ['hw'])
                TS(S, 'dve', hwk[:], p2k[:], hw[:, 0:1], None, ALU.mult, None, ['p2k', 'hw'], ['hwk'])
                TT(S, 'dve', mid[:], lo[:], hwk[:, 0:1], ALU.add, ['lo', 'hwk'], ['mid'])
            else:
                S.op('dve', lambda e: e.memset(lo[:], -1.0e29), [], ['lo'])
            TT(S, 'dve', score[:, r0:r0 + 128], score[:, r0:r0 + 128], cmaskf[:], ALU.add,
               [dk, 'cmaskf'], [dk])
            if i >= 2:
                nd = min(4096, ((n * 7 // 16) // 128) * 128)
                na = n - nd
                cthr = (TOPK - 0.5) - 0.5 * na
                for it in range(NIT):
                    TS(S, 'dve', cjunk[:, 0:nd], score[:, 0:nd], mid[:, 0:1], 0.0, ALU.is_gt, ALU.add,
                       sk + ['mid'], ['cjunk', 'cnt'], accum=cnt[:, 0:1])
                    ACT(S, ajk[:, 0:na], score[:, nd:n], AF.Sign, sk + ['mid'], ['ajk', 'sacc'],
                        bias=mid[:, 0:1], scale=-1.0, accum=sacc[:, 0:1])
                    STT(S, uu[:], sacc[:], -0.5, cnt[:], ALU.mult, ALU.add, ['sacc', 'cnt'], ['uu'])
                    TS(S, 'dve', stp[:], uu[:], cthr, hwk[:, it:it + 1], ALU.is_ge, ALU.mult,
                       ['uu', 'hwk'], ['stp'])
                    STT(S, mid[:], mid[:], hwk[:, it + 1:it + 2], stp[:], ALU.subtract, ALU.add,
                        ['mid', 'hwk', 'stp'], ['mid'])
                TT(S, 'dve', lo[:], mid[:], hwk[:, NIT:NIT + 1], ALU.subtract, ['mid', 'hwk'], ['lo'])

        SKEW = 2
        pend = []

        def A_pv(nch):
            c2, pb2 = pend.pop(0)
            for h in range(4):
                MM(S, pO[h // 2][:, h % 2, 0:129], PT[pb2][:, h, :], Vp[:, c2, h, 0:129],
                   c2 == 0 and h % 2 == 0, c2 == nch - 1 and h % 2 == 1,
                   ['PT%d' % pb2] + VPK, ['pO%d' % (h // 2)])

        def A_group(i, kb):
            nch = i + 1
            n = nch * 128
            mb = kb % 2
            wkb = min(512, n - kb * 512)
            TS(S, 'dve', maskb[mb][:, 0:wkb], score[:, kb * 512:kb * 512 + wkb], lo[:, 0:1], None,
               ALU.is_le, None, ['score%d' % kb, 'lo'], ['maskb%d' % mb])
            for cix in range(4 * kb, min(4 * kb + 4, nch)):
                s0 = cix * 128
                off = (cix % 4) * 128
                lb = li[0] % 2
                pb = li[0] % 3
                li[0] += 1
                MM(S, pL[lb][:].rearrange("p h t -> p (h t)"), maskb[mb][:, off:off + 128], negI4[:],
                   True, False, ['maskb%d' % mb, 'negI4'], ['pL%d' % lb])
                for h in range(4):
                    MM(S, pL[lb][:, h, :], KT[:, h, s0:s0 + 128], qT_t[:, h, :], False, h == 3,
                       ['KT', 'qT_t'], ['pL%d' % lb])
                ACT(S, PT[pb][:], pL[lb][:], AF.Exp, ['pL%d' % lb], ['PT%d' % pb], scale=ATT_SCALE)
                pend.append((cix, pb))
                if len(pend) > SKEW:
                    A_pv(nch)
                yield

        def A_finish(i):
            nch = i + 1
            r0 = i * 128
            while pend:
                A_pv(nch)
            for h in range(4):
                S.op('dve', lambda e, h=h: e.reciprocal(rs[:, h:h + 1], pO[h // 2][:, h % 2, 128:129]),
                     ['pO%d' % (h // 2)], ['rs'])
            for h in range(4):
                TS(S, 'dve', att_o[:, h * 128:(h + 1) * 128], pO[h // 2][:, h % 2, 0:128], rs[:, h:h + 1],
                   None, ALU.mult, None, ['pO%d' % (h // 2), 'rs'], ['att_o'])
            ab = i % 2
            ACT(S, att_b[ab][:], att_o[:], AF.Square, ['att_o'], ['att_b%d' % ab, 'ass'], accum=ass[:, 0:1])
            ACT(S, arstd[:], ass[:], AF.Ln, ['ass'], ['arstd'], bias=EPS, scale=1.0 / 512)
            ACT(S, arstd[:], arstd[:], AF.Exp, ['arstd'], ['arstd'], scale=-0.5)
            ACT(S, att_b[ab][:], att_o[:], AF.Copy, ['att_o', 'arstd'], ['att_b%d' % ab], scale=arstd[:, 0:1])
            DMA(S, 'pool', mix_s[r0:r0 + 128, 0:512], att_b[ab][:], ['att_b%d' % ab], [], 'ao%d' % ab)

        load_I(0)
        for _ in I_block(0, 0):
            pass
        load_A(0)
        if NT > 1:
            load_I(1)
        B_stage(0)
        for i in range(NT):
            nkbA = (i + 4) // 4
            nkbI = (i + 5) // 4 if i + 1 < NT else 0
            for g in range(max(nkbA, nkbI)):
                gA = A_group(i, g) if g < nkbA else iter(())
                gI = I_block(i + 1, g) if g < nkbI else iter(())
                doneA = doneI = False
                while not (doneA and doneI):
                    if not doneA:
                        try:
                            next(gA)
                        except StopIteration:
                            doneA = True
                    for _ in range(2):
                        if not doneI:
                            try:
                                next(gI)
                            except StopIteration:
                                doneI = True
            A_finish(i)
            if i + 1 < NT:
                load_A(i + 1)
                if i + 2 < NT:
                    load_I(i + 2)
                B_stage(i + 1)
        S.emit("p2")


def phase3(nc, S, L, x, w_out, w_gate_up, w_down, mixg_pm, fing_bc, modT, AB, identb, identf, onesf,
           mix_s, out):
    NS3 = L // 256
    with ExitStack() as ph3:
        def sbw(name, shape, dt):
            return ph3.enter_context(nc.sbuf_tensor(name, list(shape), dt))

        wo = sbw("wo", [128, 8, D], BF16)
        wgu = sbw("wgu", [128, 8, 2 * DFF], BF16)
        wd = sbw("wd", [128, 22, D], BF16)
        g1_bc = sbw("g1_bc", [128, D], F32)
        g2_bc = sbw("g2_bc", [128, D], F32)
        fing = sbw("fing", [128, D], F32)
        mixg = sbw("mixg", [128, 8], F32)

        with ExitStack() as ph:
            def sbp(name, shape, dt):
                return ph.enter_context(nc.sbuf_tensor(name, list(shape), dt))

            def psp(name, shape, dt):
                return ph.enter_context(nc.psum_tensor(name, list(shape), dt))

            stg = [sbp("stg3_%d" % i, [128, 2 * DFF], F32) for i in range(2)]
            tmpf = sbp("tmpf", [128, 128], F32)
            pB = psp("pB", [128, 128], F32)
            si = [0]

            def load_cast(dst, src_rows, ncols, key):
                b = si[0] % 2
                si[0] += 1
                DMA(S, 'sp' if b == 0 else 'pool', stg[b][:, 0:ncols], src_rows, [], ['stg%d' % b],
                    'stg%d' % b)
                h1 = ncols // 2
                CP(S, 'dve', dst[:, 0:h1], stg[b][:, 0:h1], ['stg%d' % b], [key + 'a'])
                CP(S, 'act', dst[:, h1:ncols], stg[b][:, h1:ncols], ['stg%d' % b], [key + 'b'])

            for k in range(8):
                load_cast(wgu[:, k, :], w_gate_up[k * 128:(k + 1) * 128, :], 2 * DFF, 'wgu%d' % k)
            for k in range(8):
                load_cast(wo[:, k, :], w_out[k * 128:(k + 1) * 128, :], D, 'wo%d' % k)
            for k in range(22):
                load_cast(wd[:, k, :], w_down[k * 128:(k + 1) * 128, :], D, 'wd%d' % k)
            DMA(S, 'sp', fing[:], fing_bc[:, :], [], ['fing'], 'fing')
            DMA(S, 'sp', mixg[:], mixg_pm[:, :], [], ['mixg'], 'mixg')
            for (dst, c0, nm) in ((g1_bc, 16, 'g1_bc'), (g2_bc, 40, 'g2_bc')):
                for k in range(8):
                    TS(S, 'dve', tmpf[:], onesf[:], modT[:, c0 + k:c0 + k + 1], None, ALU.mult, None,
                       ['onesf', 'modT'], ['tmpf'])
                    MM(S, pB[:, :], tmpf[:], identf[:], True, True, ['tmpf', 'identf'], ['pB'])
                    CP(S, 'dve', dst[:, k * 128:(k + 1) * 128], pB[:, :], ['pB'], [nm])
            S.emit("p3a")

        with ExitStack() as ph:
            def sbp(name, shape, dt):
                return ph.enter_context(nc.sbuf_tensor(name, list(shape), dt))

            def psp(name, shape, dt):
                return ph.enter_context(nc.psum_tensor(name, list(shape), dt))

            x_sb = sbp("x3", [128, 2, D], F32)
            mixb = sbp("mixb", [128, 2, D], BF16)
            mixT = sbp("mixT", [128, 8, 256], BF16)
            h2T = sbp("h2T", [128, 8, 256], BF16)
            actT = sbp("actT", [128, 22, 256], BF16)
            tmp5 = [sbp("tmp5_%d" % i, [128, 512], F32) for i in range(2)]
            ss = sbp("ss3", [128, 2], F32)
            rstd = sbp("rstd3", [128, 2], F32)
            xn = [sbp("xn3_%d" % i, [128, D], BF16) for i in range(2)]
            eg = [sbp("eg%d" % i, [128, 256], F32) for i in range(2)]

            pT = [psp("p3T%d" % i, [128, D], BF16) for i in range(2)]
            pA = [psp("p3A%d" % i, [128, 512], F32) for i in range(2)]
            pG = [psp("p3G%d" % i, [128, 512], F32) for i in range(2)]
            pD = [psp("p3D%d" % i, [128, 512], F32) for i in range(2)]
            WO = ['wo%d%s' % (k, a) for k in range(8) for a in 'ab']
            WGU = ['wgu%d%s' % (k, a) for k in range(8) for a in 'ab']
            WD = ['wd%d%s' % (k, a) for k in range(22) for a in 'ab']
            ti = [0]
            gi = [0]

            def rms_stats(j, b):
                ACT(S, xn[b][:], x_sb[:, j, :], AF.Square, ['x3'], ['xn3_%d' % b, 'ss3'], accum=ss[:, j:j + 1])
                ACT(S, rstd[:, j:j + 1], ss[:, j:j + 1], AF.Ln, ['ss3'], ['rstd3'], bias=EPS, scale=1.0 / D)
                ACT(S, rstd[:, j:j + 1], rstd[:, j:j + 1], AF.Exp, ['rstd3'], ['rstd3'], scale=-0.5)

            for st in range(NS3):
                t0 = st * 256
                DMA(S, 'sp', x_sb[:], x[t0:t0 + 256, :].rearrange("(j p) d -> p j d", p=128), [], ['x3'], 'x3')
                DMA(S, 'sp', mixb[:], mix_s[t0:t0 + 256, :].rearrange("(j p) d -> p j d", p=128), [],
                    ['mixb'], 'mixb')
                for j in range(2):
                    b = ti[0] % 2
                    ti[0] += 1
                    for k in range(8):
                        TR(S, pT[b][:, k * 128:(k + 1) * 128], mixb[:, j, k * 128:(k + 1) * 128], identb[:],
                           ['mixb', 'identb'], ['p3T%d' % b])
                    for k in range(8):
                        TS(S, 'dve', mixT[:, k, j * 128:(j + 1) * 128], pT[b][:, k * 128:(k + 1) * 128],
                           mixg[:, k:k + 1], None, ALU.mult, None, ['p3T%d' % b, 'mixg'], ['mixT'])
                for j in range(2):
                    for nb in range(2):
                        cs = slice(nb * 512, (nb + 1) * 512)
                        for k in range(8):
                            MM(S, pA[nb][:, :], mixT[:, k, j * 128:(j + 1) * 128], wo[:, k, cs], k == 0, k == 7,
                               ['mixT'] + WO, ['p3A%d' % nb])
                        TT(S, 'dve', tmp5[nb][:], pA[nb][:, :], g1_bc[:, cs], ALU.mult,
                           ['p3A%d' % nb, 'g1_bc'], ['tmp5_%d' % nb])
                        TT(S, 'pool', x_sb[:, j, cs], x_sb[:, j, cs], tmp5[nb][:], ALU.add,
                           ['x3', 'tmp5_%d' % nb], ['x3'])
                for j in range(2):
                    b = ti[0] % 2
                    ti[0] += 1
                    rms_stats(j, b)
                    ACT(S, xn[b][:], x_sb[:, j, :], AF.Copy, ['x3', 'rstd3'], ['xn3_%d' % b],
                        scale=rstd[:, j:j + 1])
                    for k in range(8):
                        TR(S, pT[b][:, k * 128:(k + 1) * 128], xn[b][:, k * 128:(k + 1) * 128], identb[:],
                           ['xn3_%d' % b, 'identb'], ['p3T%d' % b])
                    for k in range(8):
                        TS(S, 'dve', h2T[:, k, j * 128:(j + 1) * 128], pT[b][:, k * 128:(k + 1) * 128],
                           AB[:, 16 + k:17 + k], AB[:, 24 + k:25 + k], ALU.mult, ALU.add,
                           ['p3T%d' % b, 'AB2', 'AB3'], ['h2T'])
                for f in range(22):
                    g = gi[0] % 2
                    gi[0] += 1
                    for k in range(8):
                        MM(S, pG[g][:, 0:256], wgu[:, k, f * 128:(f + 1) * 128], h2T[:, k, :], k == 0, k == 7,
                           ['h2T'] + WGU, ['p3G%d' % g])
                    for k in range(8):
                        MM(S, pG[g][:, 256:512], wgu[:, k, DFF + f * 128:DFF + (f + 1) * 128], h2T[:, k, :],
                           k == 0, k == 7, ['h2T'] + WGU, ['p3G%d' % g])
                    ACT(S, eg[g][:], pG[g][:, 0:256], AF.Silu, ['p3G%d' % g], ['eg%d' % g])
                    TT(S, 'dve', actT[:, f, :], pG[g][:, 256:512], eg[g][:], ALU.mult,
                       ['p3G%d' % g, 'eg%d' % g], ['actT'])
                for j in range(2):
                    for nb in range(2):
                        cs = slice(nb * 512, (nb + 1) * 512)
                        for f in range(22):
                            MM(S, pD[nb][:, :], actT[:, f, j * 128:(j + 1) * 128], wd[:, f, cs], f == 0, f == 21,
                               ['actT'] + WD, ['p3D%d' % nb])
                        TT(S, 'dve', tmp5[nb][:], pD[nb][:, :], g2_bc[:, cs], ALU.mult,
                           ['p3D%d' % nb, 'g2_bc'], ['tmp5_%d' % nb])
                        TT(S, 'pool', x_sb[:, j, cs], x_sb[:, j, cs], tmp5[nb][:], ALU.add,
                           ['x3', 'tmp5_%d' % nb], ['x3'])
                    b = ti[0] % 2
                    ti[0] += 1
                    rms_stats(j, b)
                    ACT(S, x_sb[:, j, :], x_sb[:, j, :], AF.Copy, ['x3', 'rstd3'], ['x3'], scale=rstd[:, j:j + 1])
                    TT(S, 'pool', x_sb[:, j, :], x_sb[:, j, :], fing[:], ALU.mult, ['x3', 'fing'], ['x3'])
                    DMA(S, 'pool', out[t0 + j * 128:t0 + (j + 1) * 128, :], x_sb[:, j, :], ['x3'], [], 'yo%d' % j)
            S.emit("p3b")
```

```python
from contextlib import ExitStack

import numpy as np
import ml_dtypes
import concourse.bass as bass
import concourse.mybir as mybir
from concourse.bass_utils import run_bass_kernel_spmd

F32 = mybir.dt.float32
BF16 = mybir.dt.bfloat16
U8 = mybir.dt.uint8
AF = mybir.ActivationFunctionType
ALU = mybir.AluOpType

D = 1024
DIN = 3672
DFF = 2816
NEG = -1.0e30
EPS = 1e-6
IDX_SCALE = (8 ** -0.5) * (64 ** -0.5)
ATT_SCALE = 128 ** -0.5
CUT = [0]
NIT = 16
TOPK = 256

C_Q, C_K, C_V, C_QI, C_KI, C_WI, C_GQ, C_GK, C_GV, C_GO, C_LR = (
    0, 512, 1024, 1536, 2048, 2112, 2120, 2376, 2632, 3144, 3656)


class Sched:
    def __init__(self, nc):
        self.nc = nc
        self.ops = []
        self.state = {}
        self.slot_last = {}

    def op(self, eng, fn, reads=(), writes=(), slot=None):
        i = len(self.ops)
        deps = set()
        for b in reads:
            st = self.state.get(b)
            if st and st[0] is not None:
                deps.add(st[0])
        for b in writes:
            st = self.state.get(b)
            if st:
                if st[0] is not None:
                    deps.add(st[0])
                deps.update(st[1])
        if slot is not None:
            p = self.slot_last.get(slot)
            if p is not None:
                deps.add(p)
            self.slot_last[slot] = i
        for b in writes:
            self.state[b] = [i, []]
        for b in reads:
            st = self.state.setdefault(b, [None, []])
            st[1].append(i)
        deps.discard(i)
        self.ops.append(dict(eng=eng, fn=fn, deps=deps, slot=slot))
        return i

    def emit(self, name):
        nc = self.nc
        ops = self.ops
        needed = set()
        for o in ops:
            nd = set()
            for d in o['deps']:
                od = ops[d]
                if (od['eng'] == 'pe' and o['eng'] == 'pe'
                        and od['slot'] is None and o['slot'] is None):
                    continue
                nd.add(d)
            o['deps'] = nd
            needed |= nd
        cnt = {}
        for i, o in enumerate(ops):
            if o['slot'] is not None:
                k = 'dma_' + o['slot']
                cnt[k] = cnt.get(k, 0) + 16
                o['tok'] = (k, cnt[k])
            elif i in needed:
                k = o['eng']
                cnt[k] = cnt.get(k, 0) + 1
                o['tok'] = (k, cnt[k])
            else:
                o['tok'] = None
        waited = {}
        per_eng = {}
        for o in ops:
            w = {}
            for d in o['deps']:
                k, v = ops[d]['tok']
                if v > w.get(k, 0):
                    w[k] = v
            e = o['eng']
            wl = []
            for k, v in sorted(w.items()):
                if v > waited.get((e, k), 0):
                    waited[(e, k)] = v
                    wl.append((k, v))
            o['waits'] = wl
            per_eng.setdefault(e, []).append(o)
        with ExitStack() as es:
            sems = {k: es.enter_context(nc.semaphore(name + '_' + k)) for k in sorted(cnt)}
            blk = es.enter_context(nc.Block())

            def body(engname):
                def f(e):
                    for o in per_eng.get(engname, []):
                        for k, v in o['waits']:
                            e.wait_ge(sems[k], v)
                        ins = o['fn'](e)
                        if o['tok'] is not None:
                            ins.then_inc(sems[o['tok'][0]], 16 if o['slot'] is not None else 1)
                    if engname == 'sp':
                        for k in sorted(cnt):
                            if k.startswith('dma_'):
                                e.wait_ge(sems[k], cnt[k])
                return f

            blk.tensor(body('pe'))
            blk.scalar(body('act'))
            blk.vector(body('dve'))
            blk.gpsimd(body('pool'))
            blk.sync(body('sp'))
        self.ops = []
        self.state = {}
        self.slot_last = {}


def MM(S, out, lhsT, rhs, start, stop, R, W):
    S.op('pe', lambda e: e.matmul(out, lhsT, rhs, start=start, stop=stop), R, W)


def TR(S, out, in_, ident, R, W):
    S.op('pe', lambda e: e.transpose(out, in_, ident), R, W)


def ACT(S, out, in_, func, R, W, bias=None, scale=None, accum=None):
    kw = {}
    if bias is not None:
        kw['bias'] = bias
    if scale is not None:
        kw['scale'] = scale
    if accum is not None:
        kw['accum_out'] = accum
    S.op('act', lambda e: e.activation(out, in_, func, **kw), R, W)


def TS(S, eng, out, in0, s1, s2, op0, op1, R, W, accum=None):
    if op1 is None:
        if accum is None:
            S.op(eng, lambda e: e.tensor_scalar(out, in0, s1, None, op0), R, W)
        else:
            S.op(eng, lambda e: e.tensor_scalar(out, in0, s1, None, op0, accum_out=accum), R, W)
    else:
        if accum is None:
            S.op(eng, lambda e: e.tensor_scalar(out, in0, s1, s2, op0, op1), R, W)
        else:
            S.op(eng, lambda e: e.tensor_scalar(out, in0, s1, s2, op0, op1, accum_out=accum), R, W)


def TT(S, eng, out, in0, in1, op, R, W):
    S.op(eng, lambda e: e.tensor_tensor(out, in0, in1, op), R, W)


def STT(S, out, in0, scalar, in1, op0, op1, R, W):
    S.op('dve', lambda e: e.scalar_tensor_tensor(out, in0, scalar, in1, op0, op1), R, W)


def CP(S, eng, out, in_, R, W):
    if eng == 'act':
        S.op('act', lambda e: e.activation(out, in_, AF.Copy), R, W)
    else:
        S.op(eng, lambda e: e.tensor_copy(out, in_), R, W)


def DMA(S, eng, out, in_, R, W, slot):
    S.op(eng, lambda e: e.dma_start(out=out, in_=in_), R, W, slot=slot)


def build_nc(L, debug=False, phases=(0, 1, 2, 3)):
    NT = L // 128
    NS1 = L // 512
    nc = bass.Bass("TRN2", target_bir_lowering=False)

    def din(name, shape, dt=F32):
        return nc.dram_tensor(name, list(shape), dt, kind="ExternalInput").ap()

    x = din("x", [L, D])
    c_pm = din("c_pm", [128, 8])
    w_mod = din("w_mod", [D, 6 * D])
    b_mod_pm = din("b_mod_pm", [128, 48])
    n1g_pm = din("n1g_pm", [128, 8])
    w_in = din("w_in", [D, DIN])
    w_gate2 = din("w_gate2", [16, 256])
    b_gate2_pm = din("b_gate2_pm", [128, 2])
    mixg_pm = din("mixg_pm", [128, 8])
    w_out = din("w_out", [D, D])
    n2g_pm = din("n2g_pm", [128, 8])
    w_gate_up = din("w_gate_up", [D, 2 * DFF])
    w_down = din("w_down", [DFF, D])
    fing_bc = din("fing_bc", [128, D])
    cf32 = din("cf32", [128, 768])
    cbf = din("cbf", [128, 1152], BF16)

    okind = "ExternalOutput"
    skind = "ExternalOutput" if debug else "Internal"
    out = nc.dram_tensor("out", [L, D], F32, kind=okind).ap()

    def scr(name, shape, dt):
        return nc.dram_tensor(name, list(shape), dt, kind=skind).ap()

    qT_s = scr("qT_s", [4, 128, L], BF16)
    kT_s = scr("kT_s", [4, 128, L], BF16)
    v_s = scr("v_s", [L, 512], BF16)
    qiT_s = scr("qiT_s", [4, 128, L], BF16)
    kiT_s = scr("kiT_s", [64, L], BF16)
    wab_s = scr("wab_s", [L, 16], F32)
    mix_s = scr("mix_s", [L, 1024], BF16)

    S = Sched(nc)

    with ExitStack() as top:
        def sb(name, shape, dt):
            return top.enter_context(nc.sbuf_tensor(name, list(shape), dt))

        modT = sb("modT", [128, 48], F32)
        AB = sb("AB", [128, 32], F32)
        identb = sb("identb", [128, 128], BF16)
        tri4b = sb("tri4b", [128, 512], BF16)
        identf = sb("identf", [128, 128], F32)
        cmaskf = sb("cmaskf", [128, 128], F32)
        onesf = sb("onesf", [128, 128], F32)

        with ExitStack() as ph:
            def sbp(name, shape, dt):
                return ph.enter_context(nc.sbuf_tensor(name, list(shape), dt))

            def psp(name, shape, dt):
                return ph.enter_context(nc.psum_tensor(name, list(shape), dt))

            c_sb = sbp("c_sb", [128, 8], F32)
            sc_sb = sbp("sc_sb", [128, 8], F32)
            tmp8 = sbp("tmp8", [128, 8], F32)
            bm_sb = sbp("bm_sb", [128, 48], F32)
            g_sb = sbp("g_sb", [128, 16], F32)
            wm = [sbp("wm%d" % i, [128, 6 * D], F32) for i in range(2)]
            ps0 = psp("ps0", [128, 48], F32)

            DMA(S, 'sp', c_sb[:], c_pm[:, :], [], ['c_sb'], 'a')
            DMA(S, 'sp', bm_sb[:], b_mod_pm[:, :], [], ['bm_sb'], 'b')
            DMA(S, 'sp', g_sb[:, 0:8], n1g_pm[:, :], [], ['g_sb'], 'c')
            DMA(S, 'sp', g_sb[:, 8:16], n2g_pm[:, :], [], ['g_sb2'], 'd')
            DMA(S, 'pool', identb[:], cbf[:, 0:128], [], ['identb'], 'e')
            DMA(S, 'pool', tri4b[:], cbf[:, 128:640], [], ['tri4b'], 'f')
            DMA(S, 'pool', identf[:], cf32[:, 0:128], [], ['identf'], 'g')
            DMA(S, 'pool', cmaskf[:], cf32[:, 128:256], [], ['cmaskf'], 'h')
            DMA(S, 'pool', onesf[:], cf32[:, 256:384], [], ['onesf'], 'i')
            ACT(S, tmp8[:], c_sb[:], AF.Exp, ['c_sb'], ['tmp8'], scale=-1.0)
            TS(S, 'dve', tmp8[:], tmp8[:], 1.0, None, ALU.add, None, ['tmp8'], ['tmp8'])
            S.op('dve', lambda e: e.reciprocal(tmp8[:], tmp8[:]), ['tmp8'], ['tmp8'])
            TT(S, 'dve', sc_sb[:], c_sb[:], tmp8[:], ALU.mult, ['c_sb', 'tmp8'], ['sc_sb'])
            for k in range(8):
                b = k % 2
                DMA(S, 'sp' if b == 0 else 'pool', wm[b][:], w_mod[k * 128:(k + 1) * 128, :],
                    [], ['wm%d' % b], 'wm%d' % b)
                for j in range(48):
                    MM(S, ps0[:, j:j + 1], wm[b][:, j * 128:(j + 1) * 128], sc_sb[:, k:k + 1],
                       True, True, ['wm%d' % b, 'sc_sb'], ['ps0'])
                TT(S, 'dve', modT[:], ps0[:], bm_sb[:] if k == 0 else modT[:], ALU.add,
                   ['ps0', 'bm_sb', 'modT'], ['modT'])
            STT(S, AB[:, 0:8], modT[:, 8:16], 1.0, g_sb[:, 0:8], ALU.add, ALU.mult,
                ['modT', 'g_sb'], ['AB'])
            CP(S, 'dve', AB[:, 8:16], modT[:, 0:8], ['modT'], ['AB1'])
            STT(S, AB[:, 16:24], modT[:, 32:40], 1.0, g_sb[:, 8:16], ALU.add, ALU.mult,
                ['modT', 'g_sb2'], ['AB2'])
            CP(S, 'dve', AB[:, 24:32], modT[:, 24:32], ['modT'], ['AB3'])
            S.emit("p0")

        if 1 in phases:
            phase1(nc, S, L, x, w_in, w_gate2, b_gate2_pm, AB, identb, tri4b, identf, onesf,
                   qT_s, kT_s, v_s, qiT_s, kiT_s, wab_s, mix_s)
        if 2 in phases:
            phase2(nc, S, L, cbf, cmaskf, qT_s, kT_s, v_s, qiT_s, kiT_s, wab_s, mix_s)
        if 3 in phases:
            phase3(nc, S, L, x, w_out, w_gate_up, w_down, mixg_pm, fing_bc, modT, AB, identb, identf,
                   onesf, mix_s, out)
    return nc


def phase1(nc, S, L, x, w_in, w_gate2, b_gate2_pm, AB, identb, tri4b, identf, onesf,
           qT_s, kT_s, v_s, qiT_s, kiT_s, wab_s, mix_s):
    NS1 = L // 512
    with ExitStack() as ph:
        def sbp(name, shape, dt):
            return ph.enter_context(nc.sbuf_tensor(name, list(shape), dt))

        def psp(name, shape, dt):
            return ph.enter_context(nc.psum_tensor(name, list(shape), dt))

        wbf = sbp("wbf", [128, 8, DIN], BF16)
        stg = [sbp("stg%d" % i, [128, DIN], F32) for i in range(2)]
        wg2 = sbp("wg2", [16, 256], F32)
        bg2 = sbp("bg2", [128, 2], F32)
        nbg2 = sbp("nbg2", [128, 2], F32)
        x_sb = sbp("x_sb", [128, 4, D], F32)
        junk = sbp("junk", [128, D], BF16)
        ss = sbp("ss", [128, 4], F32)
        rstd = sbp("rstd", [128, 4], F32)
        xn = [sbp("xn%d" % i, [128, D], BF16) for i in range(2)]
        hT = sbp("hT", [128, 8, 512], BF16)
        fmo = [sbp("fmo%d" % i, [128, 512], BF16) for i in range(4)]
        gqT = sbp("gqT", [128, 2, 512], F32)
        gkT = sbp("gkT", [128, 2, 512], F32)
        lrT = sbp("lrT", [16, 512], F32)
        lneg = sbp("lneg", [128, 2, 512], F32)
        ccum = sbp("ccum", [128, 2, 512], F32)
        v_tm = sbp("v_tm", [128, 4, 512], BF16)
        gv_tm = sbp("gv_tm", [128, 4, 512], BF16)
        sgo = sbp("sgo", [128, 4, 512], F32)
        wab = sbp("wab", [128, 4, 16], F32)
        Eq = sbp("Eq", [128, 2, 128], F32)
        Ek = sbp("Ek", [128, 2, 128], F32)
        Ed = sbp("Ed", [128, 2, 128], F32)
        nb = sbp("nb", [128, 2], F32)
        EL = sbp("EL", [128, 2], F32)
        qeT = sbp("qeT", [128, 2, 128], BF16)
        keM = sbp("keM", [128, 4, 128], BF16)
        kdT = sbp("kdT", [128, 2, 128], BF16)
        kd = sbp("kd", [128, 256], BF16)
        AT = sbp("AT", [128, 4, 128], BF16)
        Sf = sbp("Sf", [128, 2, 128], F32)
        Sb = sbp("Sb", [128, 4, 128], BF16)
        oss = sbp("oss", [128, 4], F32)
        orstd = sbp("orstd", [128, 4], F32)
        ojunk = sbp("ojunk", [128, 128], F32)
        glao = [sbp("glao%d" % i, [128, 512], BF16) for i in range(2)]
        ones1 = sbp("ones1", [128, 128], F32)

        pT = [psp("pT%d" % i, [128, D], BF16) for i in range(2)]
        pF = [psp("pF%d" % i, [128, 512], F32) for i in range(2)]
        pM = [psp("pM%d" % i, [128, 512], F32) for i in range(4)]

        for k in range(8):
            b = k % 2
            DMA(S, 'sp' if b == 0 else 'pool', stg[b][:], w_in[k * 128:(k + 1) * 128, :],
                [], ['stg%d' % b], 'stg%d' % b)
            CP(S, 'dve', wbf[:, k, 0:1836], stg[b][:, 0:1836], ['stg%d' % b], ['wbf%d' % k])
            CP(S, 'act', wbf[:, k, 1836:DIN], stg[b][:, 1836:DIN], ['stg%d' % b], ['wbf%d_' % k])
        WB = ['wbf%d' % k for k in range(8)] + ['wbf%d_' % k for k in range(8)]
        DMA(S, 'sp', wg2[:], w_gate2[:, :], [], ['wg2'], 'wg2')
        DMA(S, 'sp', bg2[:], b_gate2_pm[:, :], [], ['bg2'], 'bg2')
        TS(S, 'dve', nbg2[:], bg2[:], -1.0, None, ALU.mult, None, ['bg2'], ['nbg2'])
        S.op('dve', lambda e: e.memset(Sf[:], 0.0), [], ['Sf'])
        S.op('dve', lambda e: e.memset(Sb[:], 0.0), [], ['Sb'])
        S.op('dve', lambda e: e.memset(keM[:], 0.0), [], ['keM'])
        S.op('dve', lambda e: e.memset(ones1[:], 1.0), [], ['ones1'])

        fmo_i = [0]
        st_i = [0]

        def store_slot():
            st_i[0] += 1
            return 'st%d' % (st_i[0] % 8)

        for st in range(NS1):
            t0 = st * 512
            DMA(S, 'sp', x_sb[:], x[t0:t0 + 512, :].rearrange("(j p) d -> p j d", p=128),
                [], ['x_sb'], 'x')
            for j in range(4):
                ACT(S, junk[:], x_sb[:, j, :], AF.Square, ['x_sb'], ['junk', 'ss'],
                    accum=ss[:, j:j + 1])
            ACT(S, rstd[:], ss[:], AF.Ln, ['ss'], ['rstd'], bias=EPS, scale=1.0 / D)
            ACT(S, rstd[:], rstd[:], AF.Exp, ['rstd'], ['rstd'], scale=-0.5)
            for j in range(4):
                b = j % 2
                ACT(S, xn[b][:], x_sb[:, j, :], AF.Copy, ['x_sb', 'rstd'], ['xn%d' % b],
                    scale=rstd[:, j:j + 1])
                for k in range(8):
                    TR(S, pT[b][:, k * 128:(k + 1) * 128], xn[b][:, k * 128:(k + 1) * 128],
                       identb[:], ['xn%d' % b, 'identb'], ['pT%d' % b])
                for k in range(8):
                    eng = 'dve' if k % 2 == 0 else 'pool'
                    eng = 'dve'
                    TS(S, eng, hT[:, k, j * 128:(j + 1) * 128], pT[b][:, k * 128:(k + 1) * 128],
                       AB[:, k:k + 1], AB[:, 8 + k:9 + k], ALU.mult, ALU.add,
                       ['pT%d' % b, 'AB', 'AB1'], ['hT'])

            if CUT[0] == 1:
                S.emit('p1')
                return
            def fm(col, m, pidx):
                p = pF[pidx]
                for k in range(8):
                    MM(S, p[0:m, :], wbf[:, k, col:col + m], hT[:, k, :], k == 0, k == 7,
                       ['hT'] + WB, ['pF%d' % pidx])
                return p

            pi = 0
            for (col0, dst) in ((C_Q, qT_s), (C_K, kT_s), (C_QI, qiT_s)):
                for cch in range(4):
                    p = fm(col0 + cch * 128, 128, pi)
                    f = fmo_i[0] % 4
                    fmo_i[0] += 1
                    if pi == 0:
                        CP(S, 'dve', fmo[f][:], p[:, :], ['pF%d' % pi], ['fmo%d' % f])
                    else:
                        CP(S, 'act', fmo[f][:], p[:, :], ['pF%d' % pi], ['fmo%d' % f])
                    DMA(S, 'pool', dst[cch, :, t0:t0 + 512], fmo[f][:], ['fmo%d' % f], [],
                        store_slot())
                    pi ^= 1
            p = fm(C_KI, 64, pi)
            f = fmo_i[0] % 4
            fmo_i[0] += 1
            CP(S, 'dve', fmo[f][0:64, :], p[0:64, :], ['pF%d' % pi], ['fmo%d' % f])
            DMA(S, 'pool', kiT_s[:, t0:t0 + 512], fmo[f][0:64, :], ['fmo%d' % f], [], store_slot())
            pi ^= 1
            for cch in range(2):
                p = fm(C_GQ + cch * 128, 128, pi)
                CP(S, 'act', gqT[:, cch, :], p[:, :], ['pF%d' % pi], ['gqT'])
                pi ^= 1
                p = fm(C_GK + cch * 128, 128, pi)
                CP(S, 'dve', gkT[:, cch, :], p[:, :], ['pF%d' % pi], ['gkT'])
                pi ^= 1
            p = fm(C_LR, 16, pi)
            CP(S, 'dve', lrT[:], p[0:16, :], ['pF%d' % pi], ['lrT'])
            pi ^= 1
            if CUT[0] == 2:
                S.emit('p1')
                return
            for cch in range(2):
                MM(S, pF[pi][:, :], wg2[:, cch * 128:(cch + 1) * 128], lrT[:], True, True,
                   ['wg2', 'lrT'], ['pF%d' % pi])
                ACT(S, lneg[:, cch, :], pF[pi][:, :], AF.Exp, ['pF%d' % pi, 'nbg2'], ['lneg'],
                    bias=nbg2[:, cch:cch + 1], scale=-1.0)
                pi ^= 1
            ACT(S, lneg[:], lneg[:], AF.Ln, ['lneg'], ['lneg'], bias=1.0)
            for cch in range(2):
                for j in range(4):
                    S.op('dve', lambda e, cch=cch, j=j: e.tensor_tensor_scan(
                        ccum[:, cch, j * 128:(j + 1) * 128], ones1[:, :],
                        lneg[:, cch, j * 128:(j + 1) * 128], 0.0, ALU.mult, ALU.add),
                         ['lneg', 'ones1'], ['ccum'])

            if CUT[0] == 3:
                S.emit('p1')
                return
            def tm_proj(j):
                r0 = t0 + j * 128
                for gi, (col, n) in enumerate(((C_V, 512), (C_GV, 512), (C_GO, 512), (C_WI, 8))):
                    for k in range(8):
                        MM(S, pM[gi][:, 0:n], hT[:, k, j * 128:(j + 1) * 128],
                           wbf[:, k, col:col + n], k == 0, k == 7, ['hT'] + WB, ['pM%d' % gi])
                CP(S, 'act', v_tm[:, j, :], pM[0][:, :], ['pM0'], ['v_tm'])
                CP(S, 'dve', gv_tm[:, j, :], pM[1][:, :], ['pM1'], ['gv_tm'])
                ACT(S, sgo[:, j, :], pM[2][:, :], AF.Exp, ['pM2'], ['sgo'], scale=-1.0)
                TS(S, 'dve', sgo[:, j, :], sgo[:, j, :], 1.0, None, ALU.add, None, ['sgo'], ['sgo'])
                S.op('dve', lambda e, j=j: e.reciprocal(sgo[:, j, :], sgo[:, j, :]), ['sgo'], ['sgo'])
                TT(S, 'dve', sgo[:, j, :], sgo[:, j, :], pM[2][:, :], ALU.mult, ['sgo', 'pM2'], ['sgo'])
                ACT(S, wab[:, j, 0:8], pM[3][:, 0:8], AF.Abs, ['pM3'], ['wab'], scale=IDX_SCALE)
                ACT(S, wab[:, j, 8:16], pM[3][:, 0:8], AF.Sign, ['pM3'], ['wab'])

            def gla_chunk(j):
                r0 = t0 + j * 128
                cs = slice(j * 128, (j + 1) * 128)
                ACT(S, Eq[:], ccum[:, :, cs], AF.Exp, ['ccum'], ['Eq'], scale=-1.0 / 16)
                ACT(S, Ek[:], ccum[:, :, cs], AF.Exp, ['ccum'], ['Ek'], scale=1.0 / 16)
                for cch in range(2):
                    TS(S, 'dve', nb[:, cch:cch + 1], ccum[:, cch, j * 128 + 127:j * 128 + 128],
                       -1.0 / 16, None, ALU.mult, None, ['ccum'], ['nb'])
                for cch in range(2):
                    ACT(S, Ed[:, cch, :], ccum[:, cch, cs], AF.Exp, ['ccum', 'nb'], ['Ed'],
                        bias=nb[:, cch:cch + 1], scale=1.0 / 16)
                ACT(S, EL[:], nb[:], AF.Exp, ['nb'], ['EL'])
                if CUT[0] == 5:
                    S.emit('p1')
                    return
                STT(S, qeT[:], gqT[:, :, cs], 0.125, Eq[:], ALU.mult, ALU.mult,
                    ['gqT', 'Eq'], ['qeT'])
                for h in range(4):
                    ps_ = slice((h % 2) * 64, (h % 2) * 64 + 64)
                    TT(S, 'dve', keM[ps_, h, :], gkT[ps_, h // 2, cs], Ek[ps_, h // 2, :], ALU.mult,
                       ['gkT', 'Ek'], ['keM'])
                TT(S, 'dve', kdT[:], gkT[:, :, cs], Ed[:], ALU.mult, ['gkT', 'Ed'], ['kdT'])
                if CUT[0] == 6:
                    S.emit('p1')
                    return
                for cch in range(2):
                    TR(S, pT[0][:, cch * 128:(cch + 1) * 128], kdT[:, cch, :], identb[:],
                       ['kdT', 'identb'], ['pT0'])
                CP(S, 'act', kd[:], pT[0][:, 0:256], ['pT0'], ['kd'])
                if CUT[0] == 7:
                    S.emit('p1')
                    return
                for h in range(4):
                    ps_ = slice((h % 2) * 64, (h % 2) * 64 + 64)
                    MM(S, pF[0][:, h * 128:(h + 1) * 128], keM[:, h, :], qeT[:, h // 2, :],
                       True, True, ['keM', 'qeT'], ['pF0'])
                TT(S, 'dve', AT[:].rearrange("p h i -> p (h i)"), pF[0][:, :], tri4b[:], ALU.mult,
                   ['pF0', 'tri4b'], ['AT'])
                if CUT[0] == 8:
                    S.emit('p1')
                    return
                for h in range(4):
                    ps_ = slice((h % 2) * 64, (h % 2) * 64 + 64)
                    MM(S, pF[1][:, h * 128:(h + 1) * 128], AT[:, h, :],
                       gv_tm[:, j, h * 128:(h + 1) * 128], True, False, ['AT', 'gv_tm'], ['pF1'])
                    MM(S, pF[1][:, h * 128:(h + 1) * 128], qeT[:, h // 2, :], Sb[:, h, :],
                       False, True, ['qeT', 'Sb'], ['pF1'])
                if CUT[0] == 9:
                    S.emit('p1')
                    return
                for cch in range(2):
                    MM(S, pM[0][:, cch * 256:(cch + 1) * 256], kd[:, cch * 128:(cch + 1) * 128],
                       gv_tm[:, j, cch * 256:(cch + 1) * 256], True, True, ['kd', 'gv_tm'], ['pM0'])
                for h in range(4):
                    cch, hh = h // 2, h % 2
                    ps_ = slice(hh * 64, hh * 64 + 64)
                    STT(S, Sf[ps_, cch, :], Sf[ps_, cch, :], EL[ps_, cch:cch + 1],
                        pM[0][ps_, cch * 256 + hh * 128:cch * 256 + hh * 128 + 128],
                        ALU.mult, ALU.add, ['Sf', 'EL', 'pM0'], ['Sf'])
                for h in range(4):
                    ps_ = slice((h % 2) * 64, (h % 2) * 64 + 64)
                    CP(S, 'dve', Sb[ps_, h, :], Sf[ps_, h // 2, :], ['Sf'], ['Sb'])
                if CUT[0] == 10:
                    S.emit('p1')
                    return
                for h in range(4):
                    ACT(S, ojunk[:], pF[1][:, h * 128:(h + 1) * 128], AF.Square, ['pF1'],
                        ['ojunk', 'oss'], accum=oss[:, h:h + 1])
                ACT(S, orstd[:], oss[:], AF.Ln, ['oss'], ['orstd'], bias=EPS, scale=1.0 / 128)
                ACT(S, orstd[:], orstd[:], AF.Exp, ['orstd'], ['orstd'], scale=-0.5)
                g = j % 2
                for h in range(4):
                    STT(S, glao[g][:, h * 128:(h + 1) * 128], pF[1][:, h * 128:(h + 1) * 128],
                        orstd[:, h:h + 1], sgo[:, j, h * 128:(h + 1) * 128], ALU.mult, ALU.mult,
                        ['pF1', 'orstd', 'sgo'], ['glao%d' % g])
                DMA(S, 'pool', mix_s[r0:r0 + 128, 512:1024], glao[g][:], ['glao%d' % g], [],
                    store_slot())

            tm_proj(0)
            for j in range(4):
                if j + 1 < 4:
                    tm_proj(j + 1)
                else:
                    DMA(S, 'pool', v_s[t0:t0 + 512, :].rearrange("(j p) d -> p j d", p=128), v_tm[:],
                        ['v_tm'], [], store_slot())
                    DMA(S, 'pool', wab_s[t0:t0 + 512, :].rearrange("(j p) d -> p j d", p=128), wab[:],
                        ['wab'], [], store_slot())
                gla_chunk(j)
        S.emit("p1")


def make_consts():
    ident = np.eye(128, dtype=np.float32)
    jj = np.arange(128)[:, None]
    ii = np.arange(128)[None, :]
    tri = (jj <= ii).astype(np.float32)
    cmask = np.where(ii > jj, NEG, 0.0).astype(np.float32)
    ones = np.ones((128, 128), np.float32)
    cf32 = np.concatenate([ident, cmask, ones, np.zeros((128, 384), np.float32)], axis=1)
    negi = -30000.0 * ident
    cbf = np.concatenate([ident, tri, tri, tri, tri, negi, negi, negi, negi], axis=1).astype(ml_dtypes.bfloat16)
    return np.ascontiguousarray(cf32), np.ascontiguousarray(cbf)


def pm(v, n):
    return np.ascontiguousarray(np.asarray(v, np.float32).reshape(n, 128).T)


def prep_core_inputs(inp, b, L):
    cf32, cbf = make_consts()
    f = lambda a: np.ascontiguousarray(np.asarray(a, np.float32))
    mixg = np.concatenate([np.asarray(inp['att_out_g'][0], np.float32),
                           np.tile(np.asarray(inp['gla_out_g'][0], np.float32), 4)])
    return {
        "x": f(inp['x'][b, :L]),
        "c_pm": pm(inp['c'][b], 8),
        "w_mod": f(inp['w_mod'][0]),
        "b_mod_pm": pm(inp['b_mod'][0], 48),
        "n1g_pm": pm(inp['norm1_g'][0], 8),
        "w_in": f(inp['w_in'][0]),
        "w_gate2": f(inp['w_gate2'][0]),
        "b_gate2_pm": pm(inp['b_gate2'][0], 2),
        "mixg_pm": pm(mixg, 8),
        "w_out": f(inp['w_out'][0]),
        "n2g_pm": pm(inp['norm2_g'][0], 8),
        "w_gate_up": f(inp['w_gate_up'][0]),
        "w_down": f(inp['w_down'][0]),
        "fing_bc": np.ascontiguousarray(np.broadcast_to(
            np.asarray(inp['final_g'], np.float32)[None, :], (128, D))),
        "cf32": cf32,
        "cbf": cbf,
    }


_NC_CACHE = {}


def kernel(**inputs):
    L = inputs['x'].shape[1]
    B = inputs['x'].shape[0]
    if L not in _NC_CACHE:
        _NC_CACHE[L] = build_nc(L)
    nc = _NC_CACHE[L]
    in_maps = [prep_core_inputs(inputs, b, L) for b in range(B)]
    res = run_bass_kernel_spmd(nc, in_maps, core_ids=list(range(B)))
    return np.stack([np.asarray(r["out"], np.float32) for r in res.results], axis=0)


def phase2(nc, S, L, cbf, cmaskf, qT_s, kT_s, v_s, qiT_s, kiT_s, wab_s, mix_s):
    NT = L // 128
    with ExitStack() as ph:
        def sbp(name, shape, dt):
            return ph.enter_context(nc.sbuf_tensor(name, list(shape), dt))

        def psp(name, shape, dt):
            return ph.enter_context(nc.psum_tensor(name, list(shape), dt))

        KT = sbp("KT", [128, 4, L], BF16)
        negI4 = sbp("negI4", [128, 512], BF16)
        Vp = sbp("Vp", [128, NT, 4, 132], BF16)
        kiT2 = sbp("kiT2", [128, L], BF16)
        score = sbp("score", [128, L], F32)
        cjunk = sbp("cjunk", [128, 4096], U8)
        ajk = sbp("ajk", [128, 4608], U8)
        hwk = sbp("hwk", [128, NIT + 1], F32)
        p2k = sbp("p2k", [128, NIT + 1], F32)
        sacc = sbp("sacc", [128, 1], F32)
        uu = sbp("uu", [128, 1], F32)
        maskb = [sbp("maskb%d" % i, [128, 512], BF16) for i in range(2)]
        cnt4 = sbp("cnt4", [128, 4], F32)
        qT_t = sbp("qT_t", [128, 4, 128], BF16)
        qiT_m = sbp("qiT_m", [128, 8, 128], BF16)
        wab_t = sbp("wab_t", [128, 16], F32)
        Rb = [sbp("Rb%d" % i, [128, 512], F32) for i in range(2)]
        PT = [sbp("PT%d" % i, [128, 4, 128], BF16) for i in range(3)]
        lo = sbp("lo", [128, 1], F32)
        hw = sbp("hw", [128, 1], F32)
        mid = sbp("mid", [128, 1], F32)
        cnt = sbp("cnt", [128, 1], F32)
        stp = sbp("stp", [128, 1], F32)
        mx = sbp("mx", [128, 1], F32)
        att_o = sbp("att_o", [128, 512], F32)
        rs = sbp("rs", [128, 4], F32)
        ass = sbp("ass", [128, 1], F32)
        arstd = sbp("arstd", [128, 1], F32)
        att_b = [sbp("att_b%d" % i, [128, 512], BF16) for i in range(2)]

        pI = [psp("pI%d" % i, [128, 512], F32) for i in range(2)]
        pL = [psp("pL%d" % i, [128, 4, 128], F32) for i in range(2)]
        pO = [psp("pO%d" % i, [128, 2, 132], F32) for i in range(2)]

        DMA(S, 'pool', negI4[:], cbf[:, 640:1152], [], ['negI4'], 'negI4')
        DMA(S, 'sp', KT[:], kT_s.rearrange("h d t -> d h t"), [], ['KT'], 'KT')
        DMA(S, 'pool', kiT2[0:64, :], kiT_s[:, :], [], ['kiT2'], 'ki0')
        DMA(S, 'pool', kiT2[64:128, :], kiT_s[:, :], [], ['kiT2b'], 'ki1')
        S.op('dve', lambda e: e.memset(Vp[:, :, :, 128:129], 1.0), [], ['Vp1'])
        S.op('dve', lambda e: e.memset(qiT_m[:], 0.0), [], ['qiT_m0', 'qiT_m1'])
        for kk in range(NIT + 1):
            S.op('dve', lambda e, kk=kk: e.memset(p2k[:, kk:kk + 1], 0.5 ** (kk + 1)), [], ['p2k'])
        for c0 in range(0, NT, 8):
            c1 = min(NT, c0 + 8)
            for h in range(4):
                DMA(S, 'sp' if h % 2 == 0 else 'pool', Vp[:, c0:c1, h, 0:128],
                    v_s[c0 * 128:c1 * 128, h * 128:(h + 1) * 128].rearrange("(c p) d -> p c d", p=128),
                    [], ['Vp_%d_%d' % (c0, h)], 'Vp%d' % h)
        VPK = ['Vp1'] + ['Vp_%d_%d' % (c0, h) for c0 in range(0, NT, 8) for h in range(4)]

        ri = [0]
        ii = [0]
        li = [0]
        mk = [0]

        def SK(i):
            return ['score%d' % kb for kb in range((i + 4) // 4)]

        def load_I(i):
            r0 = i * 128
            DMA(S, 'sp', qiT_m[0:64, 0:8:2, :], qiT_s[:, 0:64, r0:r0 + 128].rearrange("h d t -> d h t"), [],
                ['qiT_m0'], 'qi0')
            DMA(S, 'sp', qiT_m[64:128, 1:8:2, :], qiT_s[:, 64:128, r0:r0 + 128].rearrange("h d t -> d h t"), [],
                ['qiT_m1'], 'qi1')
            DMA(S, 'sp', wab_t[:], wab_s[r0:r0 + 128, :], [], ['wab_t'], 'wab')

        def load_A(i):
            r0 = i * 128
            DMA(S, 'sp', qT_t[:], qT_s[:, :, r0:r0 + 128].rearrange("h d t -> d h t"), [], ['qT_t'], 'q')

        def I_block(i, kb):
            n = (i + 1) * 128
            wkb = min(512, n - kb * 512)
            blk = slice(kb * 512, kb * 512 + wkb)
            sk = 'score%d' % kb
            for h in range(8):
                p = ii[0] % 2
                ii[0] += 1
                r = ri[0] % 2
                ri[0] += 1
                MM(S, pI[p][:, 0:wkb], qiT_m[:, h, :], kiT2[:, blk], True, True,
                   ['qiT_m0', 'qiT_m1', 'kiT2', 'kiT2b'], ['pI%d' % p])
                ACT(S, Rb[r][:, 0:wkb], pI[p][:, 0:wkb], AF.Relu, ['pI%d' % p, 'wab_t'],
                    ['Rb%d' % r], scale=wab_t[:, h:h + 1])
                if h == 0:
                    TS(S, 'dve', score[:, blk], Rb[r][:, 0:wkb], wab_t[:, 8:9], None, ALU.mult,
                       None, ['Rb%d' % r, 'wab_t'], [sk])
                else:
                    STT(S, score[:, blk], Rb[r][:, 0:wkb], wab_t[:, 8 + h:9 + h], score[:, blk],
                        ALU.mult, ALU.add, ['Rb%d' % r, 'wab_t', sk], [sk])
                yield

        def B_stage(i):
            n = (i + 1) * 128
            r0 = i * 128
            sk = SK(i)
            dk = 'score%d' % (i // 4)
            if i >= 2:
                S.op('dve', lambda e, n=n: e.tensor_reduce(mx[:], score[:, 0:n], mybir.AxisListType.X,
                                                          ALU.max), sk, ['mx'])
                S.op('dve', lambda e, n=n: e.tensor_reduce(lo[:], score[:, 0:n], mybir.AxisListType.X,
                                                          ALU.min), sk, ['lo'])
                TS(S, 'dve', lo[:], lo[:], -1.0, None, ALU.add, None, ['lo'], ['lo'])
                TS(S, 'dve', hw[:], mx[:], lo[:, 0:1], None, ALU.subtract, None, ['mx', 'lo'], ['hw'])
                TS(S, 'dve', hwk[:], p2k[:], hw[:, 0:1], None, ALU.mult, None, ['p2k', 'hw'], ['hwk'])
                TT(S, 'dve', mid[:], lo[:], hwk[:, 0:1], ALU.add, ['lo', 'hwk'], ['mid'])
            else:
                S.op('dve', lambda e: e.memset(lo[:], -1.0e29), [], ['lo'])
            TT(S, 'dve', score[:, r0:r0 + 128], score[:, r0:r0 + 128], cmaskf[:], ALU.add,
               [dk, 'cmaskf'], [dk])
            if i >= 2:
                nd = min(4096, max(128, n - 4608, ((n * 7 // 16) // 128) * 128))
                na = n - nd
                cthr = (TOPK - 0.5) - 0.5 * na
                for it in range(NIT):
                    TS(S, 'dve', cjunk[:, 0:nd], score[:, 0:nd], mid[:, 0:1], -cthr, ALU.is_gt, ALU.add,
                       sk + ['mid'], ['cjunk', 'cnt'], accum=cnt[:, 0:1])
                    ACT(S, ajk[:, 0:na], score[:, nd:n], AF.Sign, sk + ['mid'], ['ajk', 'sacc'],
                        bias=mid[:, 0:1], scale=-1.0, accum=sacc[:, 0:1])
                    ACT(S, uu[:], sacc[:], AF.Sign, ['sacc', 'cnt'], ['uu'], bias=cnt[:, 0:1], scale=-0.5)
                    STT(S, mid[:], uu[:], hwk[:, it + 1:it + 2], mid[:], ALU.mult, ALU.add,
                        ['uu', 'hwk', 'mid'], ['mid'])
                TT(S, 'dve', lo[:], mid[:], hwk[:, NIT:NIT + 1], ALU.subtract, ['mid', 'hwk'], ['lo'])

        SKEW = 2
        pend = []

        def A_pv(nch):
            c2, pb2 = pend.pop(0)
            for h in range(4):
                MM(S, pO[h // 2][:, h % 2, 0:129], PT[pb2][:, h, :], Vp[:, c2, h, 0:129],
                   c2 == 0 and h % 2 == 0, c2 == nch - 1 and h % 2 == 1,
                   ['PT%d' % pb2] + VPK, ['pO%d' % (h // 2)])

        def A_group(i, kb):
            nch = i + 1
            n = nch * 128
            mb = kb % 2
            wkb = min(512, n - kb * 512)
            TS(S, 'dve', maskb[mb][:, 0:wkb], score[:, kb * 512:kb * 512 + wkb], lo[:, 0:1], None,
               ALU.is_le, None, ['score%d' % kb, 'lo'], ['maskb%d' % mb])
            for cix in range(4 * kb, min(4 * kb + 4, nch)):
                s0 = cix * 128
                off = (cix % 4) * 128
                lb = li[0] % 2
                pb = li[0] % 3
                li[0] += 1
                MM(S, pL[lb][:].rearrange("p h t -> p (h t)"), maskb[mb][:, off:off + 128], negI4[:],
                   True, False, ['maskb%d' % mb, 'negI4'], ['pL%d' % lb])
                for h in range(4):
                    MM(S, pL[lb][:, h, :], KT[:, h, s0:s0 + 128], qT_t[:, h, :], False, h == 3,
                       ['KT', 'qT_t'], ['pL%d' % lb])
                ACT(S, PT[pb][:], pL[lb][:], AF.Exp, ['pL%d' % lb], ['PT%d' % pb], scale=ATT_SCALE)
                pend.append((cix, pb))
                if len(pend) > SKEW:
                    A_pv(nch)
                yield

        def A_finish(i):
            nch = i + 1
            r0 = i * 128
            while pend:
                A_pv(nch)
            for h in range(4):
                S.op('dve', lambda e, h=h: e.reciprocal(rs[:, h:h + 1], pO[h // 2][:, h % 2, 128:129]),
                     ['pO%d' % (h // 2)], ['rs'])
            for h in range(4):
                TS(S, 'dve', att_o[:, h * 128:(h + 1) * 128], pO[h // 2][:, h % 2, 0:128], rs[:, h:h + 1],
                   None, ALU.mult, None, ['pO%d' % (h // 2), 'rs'], ['att_o'])
            ab = i % 2
            ACT(S, att_b[ab][:], att_o[:], AF.Square, ['att_o'], ['att_b%d' % ab, 'ass'], accum=ass[:, 0:1])
            ACT(S, arstd[:], ass[:], AF.Ln, ['ass'], ['arstd'], bias=EPS, scale=1.0 / 512)
            ACT(S, arstd[:], arstd[:], AF.Exp, ['arstd'], ['arstd'], scale=-0.5)
            ACT(S, att_b[ab][:], att_o[:], AF.Copy, ['att_o', 'arstd'], ['att_b%d' % ab], scale=arstd[:, 0:1])
            DMA(S, 'pool', mix_s[r0:r0 + 128, 0:512], att_b[ab][:], ['att_b%d' % ab], [], 'ao%d' % ab)

        load_I(0)
        for _ in I_block(0, 0):
            pass
        load_A(0)
        if NT > 1:
            load_I(1)
        B_stage(0)
        for i in range(NT):
            nkbA = (i + 4) // 4
            nkbI = (i + 5) // 4 if i + 1 < NT else 0
            for g in range(max(nkbA, nkbI)):
                gA = A_group(i, g) if g < nkbA else iter(())
                gI = I_block(i + 1, g) if g < nkbI else iter(())
                doneA = doneI = False
                while not (doneA and doneI):
                    if not doneA:
                        try:
                            next(gA)
                        except StopIteration:
                            doneA = True
                    for _ in range(2):
                        if not doneI:
                            try:
                                next(gI)
                            except StopIteration:
                                doneI = True
            A_finish(i)
            if i + 1 < NT:
                load_A(i + 1)
                if i + 2 < NT:
                    load_I(i + 2)
                B_stage(i + 1)
        S.emit("p2")


def phase3(nc, S, L, x, w_out, w_gate_up, w_down, mixg_pm, fing_bc, modT, AB, identb, identf, onesf,
           mix_s, out):
    NS3 = L // 256
    with ExitStack() as ph3:
        def sbw(name, shape, dt):
            return ph3.enter_context(nc.sbuf_tensor(name, list(shape), dt))

        wo = sbw("wo", [128, 8, D], BF16)
        wgu = sbw("wgu", [128, 8, 2 * DFF], BF16)
        wd = sbw("wd", [128, 22, D], BF16)
        g1_bc = sbw("g1_bc", [128, D], F32)
        g2_bc = sbw("g2_bc", [128, D], F32)
        fing = sbw("fing", [128, D], F32)
        mixg = sbw("mixg", [128, 8], F32)

        with ExitStack() as ph:
            def sbp(name, shape, dt):
                return ph.enter_context(nc.sbuf_tensor(name, list(shape), dt))

            def psp(name, shape, dt):
                return ph.enter_context(nc.psum_tensor(name, list(shape), dt))

            stg = [sbp("stg3_%d" % i, [128, 2 * DFF], F32) for i in range(2)]
            tmpf = sbp("tmpf", [128, 128], F32)
            pB = psp("pB", [128, 128], F32)
            si = [0]

            def load_cast(dst, src_rows, ncols, key):
                b = si[0] % 2
                si[0] += 1
                DMA(S, 'sp' if b == 0 else 'pool', stg[b][:, 0:ncols], src_rows, [], ['stg%d' % b],
                    'stg%d' % b)
                h1 = ncols // 2
                CP(S, 'dve', dst[:, 0:h1], stg[b][:, 0:h1], ['stg%d' % b], [key + 'a'])
                CP(S, 'act', dst[:, h1:ncols], stg[b][:, h1:ncols], ['stg%d' % b], [key + 'b'])

            for k in range(8):
                load_cast(wgu[:, k, :], w_gate_up[k * 128:(k + 1) * 128, :], 2 * DFF, 'wgu%d' % k)
            for k in range(8):
                load_cast(wo[:, k, :], w_out[k * 128:(k + 1) * 128, :], D, 'wo%d' % k)
            for k in range(22):
                load_cast(wd[:, k, :], w_down[k * 128:(k + 1) * 128, :], D, 'wd%d' % k)
            DMA(S, 'sp', fing[:], fing_bc[:, :], [], ['fing'], 'fing')
            DMA(S, 'sp', mixg[:], mixg_pm[:, :], [], ['mixg'], 'mixg')
            for (dst, c0, nm) in ((g1_bc, 16, 'g1_bc'), (g2_bc, 40, 'g2_bc')):
                for k in range(8):
                    TS(S, 'dve', tmpf[:], onesf[:], modT[:, c0 + k:c0 + k + 1], None, ALU.mult, None,
                       ['onesf', 'modT'], ['tmpf'])
                    MM(S, pB[:, :], tmpf[:], identf[:], True, True, ['tmpf', 'identf'], ['pB'])
                    CP(S, 'dve', dst[:, k * 128:(k + 1) * 128], pB[:, :], ['pB'], [nm])
            S.emit("p3a")

        with ExitStack() as ph:
            def sbp(name, shape, dt):
                return ph.enter_context(nc.sbuf_tensor(name, list(shape), dt))

            def psp(name, shape, dt):
                return ph.enter_context(nc.psum_tensor(name, list(shape), dt))

            x_sb = sbp("x3", [128, 2, D], F32)
            mixb = sbp("mixb", [128, 2, D], BF16)
            mixT = sbp("mixT", [128, 8, 256], BF16)
            h2T = sbp("h2T", [128, 8, 256], BF16)
            actT = sbp("actT", [128, 22, 256], BF16)
            tmp5 = [sbp("tmp5_%d" % i, [128, 512], F32) for i in range(2)]
            ss = sbp("ss3", [128, 2], F32)
            rstd = sbp("rstd3", [128, 2], F32)
            xn = [sbp("xn3_%d" % i, [128, D], BF16) for i in range(2)]
            eg = [sbp("eg%d" % i, [128, 256], F32) for i in range(2)]

            pT = [psp("p3T%d" % i, [128, D], BF16) for i in range(2)]
            pA = [psp("p3A%d" % i, [128, 512], F32) for i in range(2)]
            pG = [psp("p3G%d" % i, [128, 512], F32) for i in range(2)]
            pD = [psp("p3D%d" % i, [128, 512], F32) for i in range(2)]
            WO = ['wo%d%s' % (k, a) for k in range(8) for a in 'ab']
            WGU = ['wgu%d%s' % (k, a) for k in range(8) for a in 'ab']
            WD = ['wd%d%s' % (k, a) for k in range(22) for a in 'ab']
            ti = [0]
            gi = [0]

            def rms_stats(j, b):
                ACT(S, xn[b][:], x_sb[:, j, :], AF.Square, ['x3'], ['xn3_%d' % b, 'ss3'], accum=ss[:, j:j + 1])
                ACT(S, rstd[:, j:j + 1], ss[:, j:j + 1], AF.Ln, ['ss3'], ['rstd3'], bias=EPS, scale=1.0 / D)
                ACT(S, rstd[:, j:j + 1], rstd[:, j:j + 1], AF.Exp, ['rstd3'], ['rstd3'], scale=-0.5)

            for st in range(NS3):
                t0 = st * 256
                DMA(S, 'sp', x_sb[:], x[t0:t0 + 256, :].rearrange("(j p) d -> p j d", p=128), [], ['x3'], 'x3')
                DMA(S, 'sp', mixb[:], mix_s[t0:t0 + 256, :].rearrange("(j p) d -> p j d", p=128), [],
                    ['mixb'], 'mixb')
                for j in range(2):
                    b = ti[0] % 2
                    ti[0] += 1
                    for k in range(8):
                        TR(S, pT[b][:, k * 128:(k + 1) * 128], mixb[:, j, k * 128:(k + 1) * 128], identb[:],
                           ['mixb', 'identb'], ['p3T%d' % b])
                    for k in range(8):
                        TS(S, 'dve', mixT[:, k, j * 128:(j + 1) * 128], pT[b][:, k * 128:(k + 1) * 128],
                           mixg[:, k:k + 1], None, ALU.mult, None, ['p3T%d' % b, 'mixg'], ['mixT'])
                for j in range(2):
                    for nb in range(2):
                        cs = slice(nb * 512, (nb + 1) * 512)
                        for k in range(8):
                            MM(S, pA[nb][:, :], mixT[:, k, j * 128:(j + 1) * 128], wo[:, k, cs], k == 0, k == 7,
                               ['mixT'] + WO, ['p3A%d' % nb])
                        TT(S, 'dve', tmp5[nb][:], pA[nb][:, :], g1_bc[:, cs], ALU.mult,
                           ['p3A%d' % nb, 'g1_bc'], ['tmp5_%d' % nb])
                        TT(S, 'pool', x_sb[:, j, cs], x_sb[:, j, cs], tmp5[nb][:], ALU.add,
                           ['x3', 'tmp5_%d' % nb], ['x3'])
                for j in range(2):
                    b = ti[0] % 2
                    ti[0] += 1
                    rms_stats(j, b)
                    ACT(S, xn[b][:], x_sb[:, j, :], AF.Copy, ['x3', 'rstd3'], ['xn3_%d' % b],
                        scale=rstd[:, j:j + 1])
                    for k in range(8):
                        TR(S, pT[b][:, k * 128:(k + 1) * 128], xn[b][:, k * 128:(k + 1) * 128], identb[:],
                           ['xn3_%d' % b, 'identb'], ['p3T%d' % b])
                    for k in range(8):
                        TS(S, 'dve', h2T[:, k, j * 128:(j + 1) * 128], pT[b][:, k * 128:(k + 1) * 128],
                           AB[:, 16 + k:17 + k], AB[:, 24 + k:25 + k], ALU.mult, ALU.add,
                           ['p3T%d' % b, 'AB2', 'AB3'], ['h2T'])
                for f in range(22):
                    g = gi[0] % 2
                    gi[0] += 1
                    for k in range(8):
                        MM(S, pG[g][:, 0:256], wgu[:, k, f * 128:(f + 1) * 128], h2T[:, k, :], k == 0, k == 7,
                           ['h2T'] + WGU, ['p3G%d' % g])
                    for k in range(8):
                        MM(S, pG[g][:, 256:512], wgu[:, k, DFF + f * 128:DFF + (f + 1) * 128], h2T[:, k, :],
                           k == 0, k == 7, ['h2T'] + WGU, ['p3G%d' % g])
                    ACT(S, eg[g][:], pG[g][:, 0:256], AF.Silu, ['p3G%d' % g], ['eg%d' % g])
                    TT(S, 'dve', actT[:, f, :], pG[g][:, 256:512], eg[g][:], ALU.mult,
                       ['p3G%d' % g, 'eg%d' % g], ['actT'])
                for j in range(2):
                    for nb in range(2):
                        cs = slice(nb * 512, (nb + 1) * 512)
                        for f in range(22):
                            MM(S, pD[nb][:, :], actT[:, f, j * 128:(j + 1) * 128], wd[:, f, cs], f == 0, f == 21,
                               ['actT'] + WD, ['p3D%d' % nb])
                        TT(S, 'dve', tmp5[nb][:], pD[nb][:, :], g2_bc[:, cs], ALU.mult,
                           ['p3D%d' % nb, 'g2_bc'], ['tmp5_%d' % nb])
                        TT(S, 'pool', x_sb[:, j, cs], x_sb[:, j, cs], tmp5[nb][:], ALU.add,
                           ['x3', 'tmp5_%d' % nb], ['x3'])
                    b = ti[0] % 2
                    ti[0] += 1
                    rms_stats(j, b)
                    ACT(S, x_sb[:, j, :], x_sb[:, j, :], AF.Copy, ['x3', 'rstd3'], ['x3'], scale=rstd[:, j:j + 1])
                    TT(S, 'pool', x_sb[:, j, :], x_sb[:, j, :], fing[:], ALU.mult, ['x3', 'fing'], ['x3'])
                    DMA(S, 'pool', out[t0 + j * 128:t0 + (j + 1) * 128, :], x_sb[:, j, :], ['x3'], [], 'yo%d' % j)
            S.emit("p3b")
```

```python
from contextlib import ExitStack

import numpy as np
import ml_dtypes
import concourse.bass as bass
import concourse.mybir as mybir
from concourse.bass_utils import run_bass_kernel_spmd

F32 = mybir.dt.float32
BF16 = mybir.dt.bfloat16
U8 = mybir.dt.uint8
AF = mybir.ActivationFunctionType
ALU = mybir.AluOpType

D = 1024
DIN = 3672
DFF = 2816
NEG = -1.0e30
EPS = 1e-6
IDX_SCALE = (8 ** -0.5) * (64 ** -0.5)
ATT_SCALE = 128 ** -0.5
CUT = [0]
NIT = 16
TOPK = 256

C_Q, C_K, C_V, C_QI, C_KI, C_WI, C_GQ, C_GK, C_GV, C_GO, C_LR = (
    0, 512, 1024, 1536, 2048, 2112, 2120, 2376, 2632, 3144, 3656)


class Sched:
    def __init__(self, nc):
        self.nc = nc
        self.ops = []
        self.state = {}
        self.slot_last = {}

    def op(self, eng, fn, reads=(), writes=(), slot=None):
        i = len(self.ops)
        deps = set()
        for b in reads:
            st = self.state.get(b)
            if st and st[0] is not None:
                deps.add(st[0])
        for b in writes:
            st = self.state.get(b)
            if st:
                if st[0] is not None:
                    deps.add(st[0])
                deps.update(st[1])
        if slot is not None:
            p = self.slot_last.get(slot)
            if p is not None:
                deps.add(p)
            self.slot_last[slot] = i
        for b in writes:
            self.state[b] = [i, []]
        for b in reads:
            st = self.state.setdefault(b, [None, []])
            st[1].append(i)
        deps.discard(i)
        self.ops.append(dict(eng=eng, fn=fn, deps=deps, slot=slot))
        return i

    def emit(self, name):
        nc = self.nc
        ops = self.ops
        needed = set()
        for o in ops:
            nd = set()
            for d in o['deps']:
                od = ops[d]
                if (od['eng'] == 'pe' and o['eng'] == 'pe'
                        and od['slot'] is None and o['slot'] is None):
                    continue
                nd.add(d)
            o['deps'] = nd
            needed |= nd
        cnt = {}
        for i, o in enumerate(ops):
            if o['slot'] is not None:
                k = 'dma_' + o['slot']
                cnt[k] = cnt.get(k, 0) + 16
                o['tok'] = (k, cnt[k])
            elif i in needed:
                k = o['eng']
                cnt[k] = cnt.get(k, 0) + 1
                o['tok'] = (k, cnt[k])
            else:
                o['tok'] = None
        waited = {}
        per_eng = {}
        for o in ops:
            w = {}
            for d in o['deps']:
                k, v = ops[d]['tok']
                if v > w.get(k, 0):
                    w[k] = v
            e = o['eng']
            wl = []
            for k, v in sorted(w.items()):
                if v > waited.get((e, k), 0):
                    waited[(e, k)] = v
                    wl.append((k, v))
            o['waits'] = wl
            per_eng.setdefault(e, []).append(o)
        with ExitStack() as es:
            sems = {k: es.enter_context(nc.semaphore(name + '_' + k)) for k in sorted(cnt)}
            blk = es.enter_context(nc.Block())

            def body(engname):
                def f(e):
                    for o in per_eng.get(engname, []):
                        for k, v in o['waits']:
                            e.wait_ge(sems[k], v)
                        ins = o['fn'](e)
                        if o['tok'] is not None:
                            ins.then_inc(sems[o['tok'][0]], 16 if o['slot'] is not None else 1)
                    if engname == 'sp':
                        for k in sorted(cnt):
                            if k.startswith('dma_'):
                                e.wait_ge(sems[k], cnt[k])
                return f

            blk.tensor(body('pe'))
            blk.scalar(body('act'))
            blk.vector(body('dve'))
            blk.gpsimd(body('pool'))
            blk.sync(body('sp'))
        self.ops = []
        self.state = {}
        self.slot_last = {}


def MM(S, out, lhsT, rhs, start, stop, R, W):
    S.op('pe', lambda e: e.matmul(out, lhsT, rhs, start=start, stop=stop), R, W)


def TR(S, out, in_, ident, R, W):
    S.op('pe', lambda e: e.transpose(out, in_, ident), R, W)


def ACT(S, out, in_, func, R, W, bias=None, scale=None, accum=None):
    kw = {}
    if bias is not None:
        kw['bias'] = bias
    if scale is not None:
        kw['scale'] = scale
    if accum is not None:
        kw['accum_out'] = accum
    S.op('act', lambda e: e.activation(out, in_, func, **kw), R, W)


def TS(S, eng, out, in0, s1, s2, op0, op1, R, W, accum=None):
    if op1 is None:
        if accum is None:
            S.op(eng, lambda e: e.tensor_scalar(out, in0, s1, None, op0), R, W)
        else:
            S.op(eng, lambda e: e.tensor_scalar(out, in0, s1, None, op0, accum_out=accum), R, W)
    else:
        if accum is None:
            S.op(eng, lambda e: e.tensor_scalar(out, in0, s1, s2, op0, op1), R, W)
        else:
            S.op(eng, lambda e: e.tensor_scalar(out, in0, s1, s2, op0, op1, accum_out=accum), R, W)


def TT(S, eng, out, in0, in1, op, R, W):
    S.op(eng, lambda e: e.tensor_tensor(out, in0, in1, op), R, W)


def STT(S, out, in0, scalar, in1, op0, op1, R, W):
    S.op('dve', lambda e: e.scalar_tensor_tensor(out, in0, scalar, in1, op0, op1), R, W)


def CP(S, eng, out, in_, R, W):
    if eng == 'act':
        S.op('act', lambda e: e.activation(out, in_, AF.Copy), R, W)
    else:
        S.op(eng, lambda e: e.tensor_copy(out, in_), R, W)


def DMA(S, eng, out, in_, R, W, slot):
    S.op(eng, lambda e: e.dma_start(out=out, in_=in_), R, W, slot=slot)


def build_nc(L, debug=False, phases=(0, 1, 2, 3)):
    NT = L // 128
    NS1 = L // 512
    nc = bass.Bass("TRN2", target_bir_lowering=False)

    def din(name, shape, dt=F32):
        return nc.dram_tensor(name, list(shape), dt, kind="ExternalInput").ap()

    x = din("x", [L, D])
    c_pm = din("c_pm", [128, 8])
    w_mod = din("w_mod", [D, 6 * D])
    b_mod_pm = din("b_mod_pm", [128, 48])
    n1g_pm = din("n1g_pm", [128, 8])
    w_in = din("w_in", [D, DIN])
    w_gate2 = din("w_gate2", [16, 256])
    b_gate2_pm = din("b_gate2_pm", [128, 2])
    mixg_pm = din("mixg_pm", [128, 8])
    w_out = din("w_out", [D, D])
    n2g_pm = din("n2g_pm", [128, 8])
    w_gate_up = din("w_gate_up", [D, 2 * DFF])
    w_down = din("w_down", [DFF, D])
    fing_bc = din("fing_bc", [128, D])
    cf32 = din("cf32", [128, 768])
    cbf = din("cbf", [128, 1152], BF16)

    okind = "ExternalOutput"
    skind = "ExternalOutput" if debug else "Internal"
    out = nc.dram_tensor("out", [L, D], F32, kind=okind).ap()

    def scr(name, shape, dt):
        return nc.dram_tensor(name, list(shape), dt, kind=skind).ap()

    qT_s = scr("qT_s", [4, 128, L], BF16)
    kT_s = scr("kT_s", [4, 128, L], BF16)
    v_s = scr("v_s", [L, 512], BF16)
    qiT_s = scr("qiT_s", [4, 128, L], BF16)
    kiT_s = scr("kiT_s", [64, L], BF16)
    wab_s = scr("wab_s", [L, 16], F32)
    mix_s = scr("mix_s", [L, 1024], BF16)

    S = Sched(nc)

    with ExitStack() as top:
        def sb(name, shape, dt):
            return top.enter_context(nc.sbuf_tensor(name, list(shape), dt))

        modT = sb("modT", [128, 48], F32)
        AB = sb("AB", [128, 32], F32)
        identb = sb("identb", [128, 128], BF16)
        tri4b = sb("tri4b", [128, 512], BF16)
        identf = sb("identf", [128, 128], F32)
        cmaskf = sb("cmaskf", [128, 128], F32)
        onesf = sb("onesf", [128, 128], F32)

        with ExitStack() as ph:
            def sbp(name, shape, dt):
                return ph.enter_context(nc.sbuf_tensor(name, list(shape), dt))

            def psp(name, shape, dt):
                return ph.enter_context(nc.psum_tensor(name, list(shape), dt))

            c_sb = sbp("c_sb", [128, 8], F32)
            sc_sb = sbp("sc_sb", [128, 8], F32)
            tmp8 = sbp("tmp8", [128, 8], F32)
            bm_sb = sbp("bm_sb", [128, 48], F32)
            g_sb = sbp("g_sb", [128, 16], F32)
            wm = [sbp("wm%d" % i, [128, 6 * D], F32) for i in range(2)]
            ps0 = psp("ps0", [128, 48], F32)

            DMA(S, 'sp', c_sb[:], c_pm[:, :], [], ['c_sb'], 'a')
            DMA(S, 'sp', bm_sb[:], b_mod_pm[:, :], [], ['bm_sb'], 'b')
            DMA(S, 'sp', g_sb[:, 0:8], n1g_pm[:, :], [], ['g_sb'], 'c')
            DMA(S, 'sp', g_sb[:, 8:16], n2g_pm[:, :], [], ['g_sb2'], 'd')
            DMA(S, 'pool', identb[:], cbf[:, 0:128], [], ['identb'], 'e')
            DMA(S, 'pool', tri4b[:], cbf[:, 128:640], [], ['tri4b'], 'f')
            DMA(S, 'pool', identf[:], cf32[:, 0:128], [], ['identf'], 'g')
            DMA(S, 'pool', cmaskf[:], cf32[:, 128:256], [], ['cmaskf'], 'h')
            DMA(S, 'pool', onesf[:], cf32[:, 256:384], [], ['onesf'], 'i')
            ACT(S, tmp8[:], c_sb[:], AF.Exp, ['c_sb'], ['tmp8'], scale=-1.0)
            TS(S, 'dve', tmp8[:], tmp8[:], 1.0, None, ALU.add, None, ['tmp8'], ['tmp8'])
            S.op('dve', lambda e: e.reciprocal(tmp8[:], tmp8[:]), ['tmp8'], ['tmp8'])
            TT(S, 'dve', sc_sb[:], c_sb[:], tmp8[:], ALU.mult, ['c_sb', 'tmp8'], ['sc_sb'])
            for k in range(8):
                b = k % 2
                DMA(S, 'sp' if b == 0 else 'pool', wm[b][:], w_mod[k * 128:(k + 1) * 128, :],
                    [], ['wm%d' % b], 'wm%d' % b)
                for j in range(48):
                    MM(S, ps0[:, j:j + 1], wm[b][:, j * 128:(j + 1) * 128], sc_sb[:, k:k + 1],
                       True, True, ['wm%d' % b, 'sc_sb'], ['ps0'])
                TT(S, 'dve', modT[:], ps0[:], bm_sb[:] if k == 0 else modT[:], ALU.add,
                   ['ps0', 'bm_sb', 'modT'], ['modT'])
            STT(S, AB[:, 0:8], modT[:, 8:16], 1.0, g_sb[:, 0:8], ALU.add, ALU.mult,
                ['modT', 'g_sb'], ['AB'])
            CP(S, 'dve', AB[:, 8:16], modT[:, 0:8], ['modT'], ['AB1'])
            STT(S, AB[:, 16:24], modT[:, 32:40], 1.0, g_sb[:, 8:16], ALU.add, ALU.mult,
                ['modT', 'g_sb2'], ['AB2'])
            CP(S, 'dve', AB[:, 24:32], modT[:, 24:32], ['modT'], ['AB3'])
            S.emit("p0")

        if 1 in phases:
            phase1(nc, S, L, x, w_in, w_gate2, b_gate2_pm, AB, identb, tri4b, identf, onesf,
                   qT_s, kT_s, v_s, qiT_s, kiT_s, wab_s, mix_s)
        if 2 in phases:
            phase2(nc, S, L, cbf, cmaskf, qT_s, kT_s, v_s, qiT_s, kiT_s, wab_s, mix_s)
        if 3 in phases:
            phase3(nc, S, L, x, w_out, w_gate_up, w_down, mixg_pm, fing_bc, modT, AB, identb, identf,
                   onesf, mix_s, out)
    return nc


def phase1(nc, S, L, x, w_in, w_gate2, b_gate2_pm, AB, identb, tri4b, identf, onesf,
           qT_s, kT_s, v_s, qiT_s, kiT_s, wab_s, mix_s):
    NS1 = L // 512
    with ExitStack() as ph:
        def sbp(name, shape, dt):
            return ph.enter_context(nc.sbuf_tensor(name, list(shape), dt))

        def psp(name, shape, dt):
            return ph.enter_context(nc.psum_tensor(name, list(shape), dt))

        wbf = sbp("wbf", [128, 8, DIN], BF16)
        stg = [sbp("stg%d" % i, [128, DIN], F32) for i in range(2)]
        wg2 = sbp("wg2", [16, 256], F32)
        bg2 = sbp("bg2", [128, 2], F32)
        nbg2 = sbp("nbg2", [128, 2], F32)
        x_sb = sbp("x_sb", [128, 4, D], F32)
        junk = sbp("junk", [128, D], BF16)
        ss = sbp("ss", [128, 4], F32)
        rstd = sbp("rstd", [128, 4], F32)
        xn = [sbp("xn%d" % i, [128, D], BF16) for i in range(2)]
        hT = sbp("hT", [128, 8, 512], BF16)
        fmo = [sbp("fmo%d" % i, [128, 512], BF16) for i in range(4)]
        gqT = sbp("gqT", [128, 2, 512], F32)
        gkT = sbp("gkT", [128, 2, 512], F32)
        lrT = sbp("lrT", [16, 512], F32)
        lneg = sbp("lneg", [128, 2, 512], F32)
        ccum = sbp("ccum", [128, 2, 512], F32)
        v_tm = sbp("v_tm", [128, 4, 512], BF16)
        gv_tm = sbp("gv_tm", [128, 4, 512], BF16)
        sgo = sbp("sgo", [128, 4, 512], F32)
        wab = sbp("wab", [128, 4, 16], F32)
        Eq = sbp("Eq", [128, 2, 128], F32)
        Ek = sbp("Ek", [128, 2, 128], F32)
        Ed = sbp("Ed", [128, 2, 128], F32)
        nb = sbp("nb", [128, 2], F32)
        EL = sbp("EL", [128, 2], F32)
        qeT = sbp("qeT", [128, 2, 128], BF16)
        keM = sbp("keM", [128, 4, 128], BF16)
        kdT = sbp("kdT", [128, 2, 128], BF16)
        kd = sbp("kd", [128, 256], BF16)
        AT = sbp("AT", [128, 4, 128], BF16)
        Sf = sbp("Sf", [128, 2, 128], F32)
        Sb = sbp("Sb", [128, 4, 128], BF16)
        oss = sbp("oss", [128, 4], F32)
        orstd = sbp("orstd", [128, 4], F32)
        ojunk = sbp("ojunk", [128, 128], F32)
        glao = [sbp("glao%d" % i, [128, 512], BF16) for i in range(2)]
        ones1 = sbp("ones1", [128, 128], F32)

        pT = [psp("pT%d" % i, [128, D], BF16) for i in range(2)]
        pF = [psp("pF%d" % i, [128, 512], F32) for i in range(2)]
        pM = [psp("pM%d" % i, [128, 512], F32) for i in range(4)]

        for k in range(8):
            b = k % 2
            DMA(S, 'sp' if b == 0 else 'pool', stg[b][:], w_in[k * 128:(k + 1) * 128, :],
                [], ['stg%d' % b], 'stg%d' % b)
            CP(S, 'dve', wbf[:, k, 0:1836], stg[b][:, 0:1836], ['stg%d' % b], ['wbf%d' % k])
            CP(S, 'act', wbf[:, k, 1836:DIN], stg[b][:, 1836:DIN], ['stg%d' % b], ['wbf%d_' % k])
        WB = ['wbf%d' % k for k in range(8)] + ['wbf%d_' % k for k in range(8)]
        DMA(S, 'sp', wg2[:], w_gate2[:, :], [], ['wg2'], 'wg2')
        DMA(S, 'sp', bg2[:], b_gate2_pm[:, :], [], ['bg2'], 'bg2')
        TS(S, 'dve', nbg2[:], bg2[:], -1.0, None, ALU.mult, None, ['bg2'], ['nbg2'])
        S.op('dve', lambda e: e.memset(Sf[:], 0.0), [], ['Sf'])
        S.op('dve', lambda e: e.memset(Sb[:], 0.0), [], ['Sb'])
        S.op('dve', lambda e: e.memset(keM[:], 0.0), [], ['keM'])
        S.op('dve', lambda e: e.memset(ones1[:], 1.0), [], ['ones1'])

        fmo_i = [0]
        st_i = [0]

        def store_slot():
            st_i[0] += 1
            return 'st%d' % (st_i[0] % 8)

        for st in range(NS1):
            t0 = st * 512
            DMA(S, 'sp', x_sb[:], x[t0:t0 + 512, :].rearrange("(j p) d -> p j d", p=128),
                [], ['x_sb'], 'x')
            for j in range(4):
                ACT(S, junk[:], x_sb[:, j, :], AF.Square, ['x_sb'], ['junk', 'ss'],
                    accum=ss[:, j:j + 1])
            ACT(S, rstd[:], ss[:], AF.Ln, ['ss'], ['rstd'], bias=EPS, scale=1.0 / D)
            ACT(S, rstd[:], rstd[:], AF.Exp, ['rstd'], ['rstd'], scale=-0.5)
            for j in range(4):
                b = j % 2
                ACT(S, xn[b][:], x_sb[:, j, :], AF.Copy, ['x_sb', 'rstd'], ['xn%d' % b],
                    scale=rstd[:, j:j + 1])
                for k in range(8):
                    TR(S, pT[b][:, k * 128:(k + 1) * 128], xn[b][:, k * 128:(k + 1) * 128],
                       identb[:], ['xn%d' % b, 'identb'], ['pT%d' % b])
                for k in range(8):
                    eng = 'dve' if k % 2 == 0 else 'pool'
                    eng = 'dve'
                    TS(S, eng, hT[:, k, j * 128:(j + 1) * 128], pT[b][:, k * 128:(k + 1) * 128],
                       AB[:, k:k + 1], AB[:, 8 + k:9 + k], ALU.mult, ALU.add,
                       ['pT%d' % b, 'AB', 'AB1'], ['hT'])

            if CUT[0] == 1:
                S.emit('p1')
                return
            def fm(col, m, pidx):
                p = pF[pidx]
                for k in range(8):
                    MM(S, p[0:m, :], wbf[:, k, col:col + m], hT[:, k, :], k == 0, k == 7,
                       ['hT'] + WB, ['pF%d' % pidx])
                return p

            pi = 0
            for (col0, dst) in ((C_Q, qT_s), (C_K, kT_s), (C_QI, qiT_s)):
                for cch in range(4):
                    p = fm(col0 + cch * 128, 128, pi)
                    f = fmo_i[0] % 4
                    fmo_i[0] += 1
                    if pi == 0:
                        CP(S, 'dve', fmo[f][:], p[:, :], ['pF%d' % pi], ['fmo%d' % f])
                    else:
                        CP(S, 'act', fmo[f][:], p[:, :], ['pF%d' % pi], ['fmo%d' % f])
                    DMA(S, 'pool', dst[cch, :, t0:t0 + 512], fmo[f][:], ['fmo%d' % f], [],
                        store_slot())
                    pi ^= 1
            p = fm(C_KI, 64, pi)
            f = fmo_i[0] % 4
            fmo_i[0] += 1
            CP(S, 'dve', fmo[f][0:64, :], p[0:64, :], ['pF%d' % pi], ['fmo%d' % f])
            DMA(S, 'pool', kiT_s[:, t0:t0 + 512], fmo[f][0:64, :], ['fmo%d' % f], [], store_slot())
            pi ^= 1
            for cch in range(2):
                p = fm(C_GQ + cch * 128, 128, pi)
                CP(S, 'act', gqT[:, cch, :], p[:, :], ['pF%d' % pi], ['gqT'])
                pi ^= 1
                p = fm(C_GK + cch * 128, 128, pi)
                CP(S, 'dve', gkT[:, cch, :], p[:, :], ['pF%d' % pi], ['gkT'])
                pi ^= 1
            p = fm(C_LR, 16, pi)
            CP(S, 'dve', lrT[:], p[0:16, :], ['pF%d' % pi], ['lrT'])
            pi ^= 1
            if CUT[0] == 2:
                S.emit('p1')
                return
            for cch in range(2):
                MM(S, pF[pi][:, :], wg2[:, cch * 128:(cch + 1) * 128], lrT[:], True, True,
                   ['wg2', 'lrT'], ['pF%d' % pi])
                ACT(S, lneg[:, cch, :], pF[pi][:, :], AF.Exp, ['pF%d' % pi, 'nbg2'], ['lneg'],
                    bias=nbg2[:, cch:cch + 1], scale=-1.0)
                pi ^= 1
            ACT(S, lneg[:], lneg[:], AF.Ln, ['lneg'], ['lneg'], bias=1.0)
            for cch in range(2):
                for j in range(4):
                    S.op('dve', lambda e, cch=cch, j=j: e.tensor_tensor_scan(
                        ccum[:, cch, j * 128:(j + 1) * 128], ones1[:, :],
                        lneg[:, cch, j * 128:(j + 1) * 128], 0.0, ALU.mult, ALU.add),
                         ['lneg', 'ones1'], ['ccum'])

            if CUT[0] == 3:
                S.emit('p1')
                return
            def tm_proj(j):
                r0 = t0 + j * 128
                for gi, (col, n) in enumerate(((C_V, 512), (C_GV, 512), (C_GO, 512), (C_WI, 8))):
                    for k in range(8):
                        MM(S, pM[gi][:, 0:n], hT[:, k, j * 128:(j + 1) * 128],
                           wbf[:, k, col:col + n], k == 0, k == 7, ['hT'] + WB, ['pM%d' % gi])
                CP(S, 'act', v_tm[:, j, :], pM[0][:, :], ['pM0'], ['v_tm'])
                CP(S, 'dve', gv_tm[:, j, :], pM[1][:, :], ['pM1'], ['gv_tm'])
                ACT(S, sgo[:, j, :], pM[2][:, :], AF.Exp, ['pM2'], ['sgo'], scale=-1.0)
                TS(S, 'dve', sgo[:, j, :], sgo[:, j, :], 1.0, None, ALU.add, None, ['sgo'], ['sgo'])
                S.op('dve', lambda e, j=j: e.reciprocal(sgo[:, j, :], sgo[:, j, :]), ['sgo'], ['sgo'])
                TT(S, 'dve', sgo[:, j, :], sgo[:, j, :], pM[2][:, :], ALU.mult, ['sgo', 'pM2'], ['sgo'])
                ACT(S, wab[:, j, 0:8], pM[3][:, 0:8], AF.Abs, ['pM3'], ['wab'], scale=IDX_SCALE)
                ACT(S, wab[:, j, 8:16], pM[3][:, 0:8], AF.Sign, ['pM3'], ['wab'])

            def gla_chunk(j):
                r0 = t0 + j * 128
                cs = slice(j * 128, (j + 1) * 128)
                ACT(S, Eq[:], ccum[:, :, cs], AF.Exp, ['ccum'], ['Eq'], scale=-1.0 / 16)
                ACT(S, Ek[:], ccum[:, :, cs], AF.Exp, ['ccum'], ['Ek'], scale=1.0 / 16)
                for cch in range(2):
                    TS(S, 'dve', nb[:, cch:cch + 1], ccum[:, cch, j * 128 + 127:j * 128 + 128],
                       -1.0 / 16, None, ALU.mult, None, ['ccum'], ['nb'])
                for cch in range(2):
                    ACT(S, Ed[:, cch, :], ccum[:, cch, cs], AF.Exp, ['ccum', 'nb'], ['Ed'],
                        bias=nb[:, cch:cch + 1], scale=1.0 / 16)
                ACT(S, EL[:], nb[:], AF.Exp, ['nb'], ['EL'])
                if CUT[0] == 5:
                    S.emit('p1')
                    return
                STT(S, qeT[:], gqT[:, :, cs], 0.125, Eq[:], ALU.mult, ALU.mult,
                    ['gqT', 'Eq'], ['qeT'])
                for h in range(4):
                    ps_ = slice((h % 2) * 64, (h % 2) * 64 + 64)
                    TT(S, 'dve', keM[ps_, h, :], gkT[ps_, h // 2, cs], Ek[ps_, h // 2, :], ALU.mult,
                       ['gkT', 'Ek'], ['keM'])
                TT(S, 'dve', kdT[:], gkT[:, :, cs], Ed[:], ALU.mult, ['gkT', 'Ed'], ['kdT'])
                if CUT[0] == 6:
                    S.emit('p1')
                    return
                for cch in range(2):
                    TR(S, pT[0][:, cch * 128:(cch + 1) * 128], kdT[:, cch, :], identb[:],
                       ['kdT', 'identb'], ['pT0'])
                CP(S, 'act', kd[:], pT[0][:, 0:256], ['pT0'], ['kd'])
                if CUT[0] == 7:
                    S.emit('p1')
                    return
                for h in range(4):
                    ps_ = slice((h % 2) * 64, (h % 2) * 64 + 64)
                    MM(S, pF[0][:, h * 128:(h + 1) * 128], keM[:, h, :], qeT[:, h // 2, :],
                       True, True, ['keM', 'qeT'], ['pF0'])
                TT(S, 'dve', AT[:].rearrange("p h i -> p (h i)"), pF[0][:, :], tri4b[:], ALU.mult,
                   ['pF0', 'tri4b'], ['AT'])
                if CUT[0] == 8:
                    S.emit('p1')
                    return
                for h in range(4):
                    ps_ = slice((h % 2) * 64, (h % 2) * 64 + 64)
                    MM(S, pF[1][:, h * 128:(h + 1) * 128], AT[:, h, :],
                       gv_tm[:, j, h * 128:(h + 1) * 128], True, False, ['AT', 'gv_tm'], ['pF1'])
                    MM(S, pF[1][:, h * 128:(h + 1) * 128], qeT[:, h // 2, :], Sb[:, h, :],
                       False, True, ['qeT', 'Sb'], ['pF1'])
                if CUT[0] == 9:
                    S.emit('p1')
                    return
                for cch in range(2):
                    MM(S, pM[0][:, cch * 256:(cch + 1) * 256], kd[:, cch * 128:(cch + 1) * 128],
                       gv_tm[:, j, cch * 256:(cch + 1) * 256], True, True, ['kd', 'gv_tm'], ['pM0'])
                for h in range(4):
                    cch, hh = h // 2, h % 2
                    ps_ = slice(hh * 64, hh * 64 + 64)
                    STT(S, Sf[ps_, cch, :], Sf[ps_, cch, :], EL[ps_, cch:cch + 1],
                        pM[0][ps_, cch * 256 + hh * 128:cch * 256 + hh * 128 + 128],
                        ALU.mult, ALU.add, ['Sf', 'EL', 'pM0'], ['Sf'])
                for h in range(4):
                    ps_ = slice((h % 2) * 64, (h % 2) * 64 + 64)
                    CP(S, 'dve', Sb[ps_, h, :], Sf[ps_, h // 2, :], ['Sf'], ['Sb'])
                if CUT[0] == 10:
                    S.emit('p1')
                    return
                for h in range(4):
                    ACT(S, ojunk[:], pF[1][:, h * 128:(h + 1) * 128], AF.Square, ['pF1'],
                        ['ojunk', 'oss'], accum=oss[:, h:h + 1])
                ACT(S, orstd[:], oss[:], AF.Ln, ['oss'], ['orstd'], bias=EPS, scale=1.0 / 128)
                ACT(S, orstd[:], orstd[:], AF.Exp, ['orstd'], ['orstd'], scale=-0.5)
                g = j % 2
                for h in range(4):
                    STT(S, glao[g][:, h * 128:(h + 1) * 128], pF[1][:, h * 128:(h + 1) * 128],
                        orstd[:, h:h + 1], sgo[:, j, h * 128:(h + 1) * 128], ALU.mult, ALU.mult,
                        ['pF1', 'orstd', 'sgo'], ['glao%d' % g])
                DMA(S, 'pool', mix_s[r0:r0 + 128, 512:1024], glao[g][:], ['glao%d' % g], [],
                    store_slot())

            tm_proj(0)
            for j in range(4):
                if j + 1 < 4:
                    tm_proj(j + 1)
                else:
                    DMA(S, 'pool', v_s[t0:t0 + 512, :].rearrange("(j p) d -> p j d", p=128), v_tm[:],
                        ['v_tm'], [], store_slot())
                    DMA(S, 'pool', wab_s[t0:t0 + 512, :].rearrange("(j p) d -> p j d", p=128), wab[:],
                        ['wab'], [], store_slot())
                gla_chunk(j)
        S.emit("p1")


def make_consts():
    ident = np.eye(128, dtype=np.float32)
    jj = np.arange(128)[:, None]
    ii = np.arange(128)[None, :]
    tri = (jj <= ii).astype(np.float32)
    cmask = np.where(ii > jj, NEG, 0.0).astype(np.float32)
    ones = np.ones((128, 128), np.float32)
    cf32 = np.concatenate([ident, cmask, ones, np.zeros((128, 384), np.float32)], axis=1)
    negi = -30000.0 * ident
    cbf = np.concatenate([ident, tri, tri, tri, tri, negi, negi, negi, negi], axis=1).astype(ml_dtypes.bfloat16)
    return np.ascontiguousarray(cf32), np.ascontiguousarray(cbf)


def pm(v, n):
    return np.ascontiguousarray(np.asarray(v, np.float32).reshape(n, 128).T)


def prep_core_inputs(inp, b, L):
    cf32, cbf = make_consts()
    f = lambda a: np.ascontiguousarray(np.asarray(a, np.float32))
    mixg = np.concatenate([np.asarray(inp['att_out_g'][0], np.float32),
                           np.tile(np.asarray(inp['gla_out_g'][0], np.float32), 4)])
    return {
        "x": f(inp['x'][b, :L]),
        "c_pm": pm(inp['c'][b], 8),
        "w_mod": f(inp['w_mod'][0]),
        "b_mod_pm": pm(inp['b_mod'][0], 48),
        "n1g_pm": pm(inp['norm1_g'][0], 8),
        "w_in": f(inp['w_in'][0]),
        "w_gate2": f(inp['w_gate2'][0]),
        "b_gate2_pm": pm(inp['b_gate2'][0], 2),
        "mixg_pm": pm(mixg, 8),
        "w_out": f(inp['w_out'][0]),
        "n2g_pm": pm(inp['norm2_g'][0], 8),
        "w_gate_up": f(inp['w_gate_up'][0]),
        "w_down": f(inp['w_down'][0]),
        "fing_bc": np.ascontiguousarray(np.broadcast_to(
            np.asarray(inp['final_g'], np.float32)[None, :], (128, D))),
        "cf32": cf32,
        "cbf": cbf,
    }


_NC_CACHE = {}


def kernel(**inputs):
    L = inputs['x'].shape[1]
    B = inputs['x'].shape[0]
    if L not in _NC_CACHE:
        _NC_CACHE[L] = build_nc(L)
    nc = _NC_CACHE[L]
    in_maps = [prep_core_inputs(inputs, b, L) for b in range(B)]
    res = run_bass_kernel_spmd(nc, in_maps, core_ids=list(range(B)))
    return np.stack([np.asarray(r["out"], np.float32) for r in res.results], axis=0)


def phase2(nc, S, L, cbf, cmaskf, qT_s, kT_s, v_s, qiT_s, kiT_s, wab_s, mix_s):
    NT = L // 128
    with ExitStack() as ph:
        def sbp(name, shape, dt):
            return ph.enter_context(nc.sbuf_tensor(name, list(shape), dt))

        def psp(name, shape, dt):
            return ph.enter_context(nc.psum_tensor(name, list(shape), dt))

        KT = sbp("KT", [128, 4, L], BF16)
        negI4 = sbp("negI4", [128, 512], BF16)
        Vp = sbp("Vp", [128, NT, 4, 132], BF16)
        kiT2 = sbp("kiT2", [128, L], BF16)
        score = sbp("score", [128, L], F32)
        cjunk = sbp("cjunk", [128, 4096], U8)
        ajk = sbp("ajk", [128, 4608], U8)
        hwk = sbp("hwk", [128, NIT + 1], F32)
        p2k = sbp("p2k", [128, NIT + 1], F32)
        sacc = sbp("sacc", [128, 1], F32)
        uu = sbp("uu", [128, 1], F32)
        maskb = [sbp("maskb%d" % i, [128, 512], BF16) for i in range(2)]
        cnt4 = sbp("cnt4", [128, 4], F32)
        qT_t = sbp("qT_t", [128, 4, 128], BF16)
        qiT_m = sbp("qiT_m", [128, 8, 128], BF16)
        wab_t = sbp("wab_t", [128, 16], F32)
        Rb = [sbp("Rb%d" % i, [128, 512], F32) for i in range(2)]
        PT = [sbp("PT%d" % i, [128, 4, 128], BF16) for i in range(3)]
        lo = sbp("lo", [128, 1], F32)
        hw = sbp("hw", [128, 1], F32)
        mid = sbp("mid", [128, 1], F32)
        cnt = sbp("cnt", [128, 1], F32)
        stp = sbp("stp", [128, 1], F32)
        mx = sbp("mx", [128, 1], F32)
        att_o = sbp("att_o", [128, 512], F32)
        rs = sbp("rs", [128, 4], F32)
        ass = sbp("ass", [128, 1], F32)
        arstd = sbp("arstd", [128, 1], F32)
        att_b = [sbp("att_b%d" % i, [128, 512], BF16) for i in range(2)]

        pI = [psp("pI%d" % i, [128, 512], F32) for i in range(2)]
        pL = [psp("pL%d" % i, [128, 4, 128], F32) for i in range(2)]
        pO = [psp("pO%d" % i, [128, 2, 132], F32) for i in range(2)]

        DMA(S, 'pool', negI4[:], cbf[:, 640:1152], [], ['negI4'], 'negI4')
        DMA(S, 'sp', KT[:], kT_s.rearrange("h d t -> d h t"), [], ['KT'], 'KT')
        DMA(S, 'pool', kiT2[0:64, :], kiT_s[:, :], [], ['kiT2'], 'ki0')
        DMA(S, 'pool', kiT2[64:128, :], kiT_s[:, :], [], ['kiT2b'], 'ki1')
        S.op('dve', lambda e: e.memset(Vp[:, :, :, 128:129], 1.0), [], ['Vp1'])
        S.op('dve', lambda e: e.memset(qiT_m[:], 0.0), [], ['qiT_m0', 'qiT_m1'])
        for kk in range(NIT + 1):
            S.op('dve', lambda e, kk=kk: e.memset(p2k[:, kk:kk + 1], 0.5 ** (kk + 1)), [], ['p2k'])
        for c0 in range(0, NT, 8):
            c1 = min(NT, c0 + 8)
            for h in range(4):
                DMA(S, 'sp' if h % 2 == 0 else 'pool', Vp[:, c0:c1, h, 0:128],
                    v_s[c0 * 128:c1 * 128, h * 128:(h + 1) * 128].rearrange("(c p) d -> p c d", p=128),
                    [], ['Vp_%d_%d' % (c0, h)], 'Vp%d' % h)
        VPK = ['Vp1'] + ['Vp_%d_%d' % (c0, h) for c0 in range(0, NT, 8) for h in range(4)]

        ri = [0]
        ii = [0]
        li = [0]
        mk = [0]

        def SK(i):
            return ['score%d' % kb for kb in range((i + 4) // 4)]

        def load_I(i):
            r0 = i * 128
            DMA(S, 'sp', qiT_m[0:64, 0:8:2, :], qiT_s[:, 0:64, r0:r0 + 128].rearrange("h d t -> d h t"), [],
                ['qiT_m0'], 'qi0')
            DMA(S, 'sp', qiT_m[64:128, 1:8:2, :], qiT_s[:, 64:128, r0:r0 + 128].rearrange("h d t -> d h t"), [],
                ['qiT_m1'], 'qi1')
            DMA(S, 'sp', wab_t[:], wab_s[r0:r0 + 128, :], [], ['wab_t'], 'wab')

        def load_A(i):
            r0 = i * 128
            DMA(S, 'sp', qT_t[:], qT_s[:, :, r0:r0 + 128].rearrange("h d t -> d h t"), [], ['qT_t'], 'q')

        def I_block(i, kb):
            n = (i + 1) * 128
            wkb = min(512, n - kb * 512)
            blk = slice(kb * 512, kb * 512 + wkb)
            sk = 'score%d' % kb
            for h in range(8):
                p = ii[0] % 2
                ii[0] += 1
                r = ri[0] % 2
                ri[0] += 1
                MM(S, pI[p][:, 0:wkb], qiT_m[:, h, :], kiT2[:, blk], True, True,
                   ['qiT_m0', 'qiT_m1', 'kiT2', 'kiT2b'], ['pI%d' % p])
                ACT(S, Rb[r][:, 0:wkb], pI[p][:, 0:wkb], AF.Relu, ['pI%d' % p, 'wab_t'],
                    ['Rb%d' % r], scale=wab_t[:, h:h + 1])
                if h == 0:
                    TS(S, 'dve', score[:, blk], Rb[r][:, 0:wkb], wab_t[:, 8:9], None, ALU.mult,
                       None, ['Rb%d' % r, 'wab_t'], [sk])
                else:
                    STT(S, score[:, blk], Rb[r][:, 0:wkb], wab_t[:, 8 + h:9 + h], score[:, blk],
                        ALU.mult, ALU.add, ['Rb%d' % r, 'wab_t', sk], [sk])
                yield

        def B_stage(i):
            n = (i + 1) * 128
            r0 = i * 128
            sk = SK(i)
            dk = 'score%d' % (i // 4)
            if i >= 2:
                S.op('dve', lambda e, n=n: e.tensor_reduce(mx[:], score[:, 0:n], mybir.AxisListType.X,
                                                          ALU.max, apply_absolute_value=True), sk, ['mx'])
                TS(S, 'dve', lo[:], mx[:], -1.0, -1.0, ALU.mult, ALU.add, ['mx'], ['lo'])
                TS(S, 'dve', hw[:], mx[:], lo[:, 0:1], None, ALU.subtract, None, ['mx', 'lo'], ['hw'])
                TS(S, 'dve', hwk[:], p2k[:], hw[:, 0:1], None, ALU.mult, None, ['p2k', 'hw'], ['hwk'])
                TT(S, 'dve', mid[:], lo[:], hwk[:, 0:1], ALU.add, ['lo', 'hwk'], ['mid'])
            else:
                S.op('dve', lambda e: e.memset(lo[:], -1.0e29), [], ['lo'])
            TT(S, 'dve', score[:, r0:r0 + 128], score[:, r0:r0 + 128], cmaskf[:], ALU.add,
               [dk, 'cmaskf'], [dk])
            if i >= 2:
                nd = min(4096, max(128, n - 4608, ((n * 7 // 16) // 128) * 128))
                na = n - nd
                cthr = (TOPK - 0.5) - 0.5 * na
                for it in range(NIT):
                    TS(S, 'dve', cjunk[:, 0:nd], score[:, 0:nd], mid[:, 0:1], -cthr, ALU.is_gt, ALU.add,
                       sk + ['mid'], ['cjunk', 'cnt'], accum=cnt[:, 0:1])
                    ACT(S, ajk[:, 0:na], score[:, nd:n], AF.Sign, sk + ['mid'], ['ajk', 'sacc'],
                        bias=mid[:, 0:1], scale=-1.0, accum=sacc[:, 0:1])
                    ACT(S, uu[:], sacc[:], AF.Sign, ['sacc', 'cnt'], ['uu'], bias=cnt[:, 0:1], scale=-0.5)
                    STT(S, mid[:], uu[:], hwk[:, it + 1:it + 2], mid[:], ALU.mult, ALU.add,
                        ['uu', 'hwk', 'mid'], ['mid'])
                TT(S, 'dve', lo[:], mid[:], hwk[:, NIT:NIT + 1], ALU.subtract, ['mid', 'hwk'], ['lo'])

        SKEW = 2
        pend = []

        def A_pv(nch):
            c2, pb2 = pend.pop(0)
            for h in range(4):
                MM(S, pO[h // 2][:, h % 2, 0:129], PT[pb2][:, h, :], Vp[:, c2, h, 0:129],
                   c2 == 0 and h % 2 == 0, c2 == nch - 1 and h % 2 == 1,
                   ['PT%d' % pb2] + VPK, ['pO%d' % (h // 2)])

        def A_group(i, kb):
            nch = i + 1
            n = nch * 128
            mb = kb % 2
            wkb = min(512, n - kb * 512)
            TS(S, 'dve', maskb[mb][:, 0:wkb], score[:, kb * 512:kb * 512 + wkb], lo[:, 0:1], None,
               ALU.is_le, None, ['score%d' % kb, 'lo'], ['maskb%d' % mb])
            for cix in range(4 * kb, min(4 * kb + 4, nch)):
                s0 = cix * 128
                off = (cix % 4) * 128
                lb = li[0] % 2
                pb = li[0] % 3
                li[0] += 1
                MM(S, pL[lb][:].rearrange("p h t -> p (h t)"), maskb[mb][:, off:off + 128], negI4[:],
                   True, False, ['maskb%d' % mb, 'negI4'], ['pL%d' % lb])
                for h in range(4):
                    MM(S, pL[lb][:, h, :], KT[:, h, s0:s0 + 128], qT_t[:, h, :], False, h == 3,
                       ['KT', 'qT_t'], ['pL%d' % lb])
                ACT(S, PT[pb][:], pL[lb][:], AF.Exp, ['pL%d' % lb], ['PT%d' % pb], scale=ATT_SCALE)
                pend.append((cix, pb))
                if len(pend) > SKEW:
                    A_pv(nch)
                yield

        def A_finish(i):
            nch = i + 1
            r0 = i * 128
            while pend:
                A_pv(nch)
            for h in range(4):
                S.op('dve', lambda e, h=h: e.reciprocal(rs[:, h:h + 1], pO[h // 2][:, h % 2, 128:129]),
                     ['pO%d' % (h // 2)], ['rs'])
            for h in range(4):
                TS(S, 'dve', att_o[:, h * 128:(h + 1) * 128], pO[h // 2][:, h % 2, 0:128], rs[:, h:h + 1],
                   None, ALU.mult, None, ['pO%d' % (h // 2), 'rs'], ['att_o'])
            ab = i % 2
            ACT(S, att_b[ab][:], att_o[:], AF.Square, ['att_o'], ['att_b%d' % ab, 'ass'], accum=ass[:, 0:1])
            ACT(S, arstd[:], ass[:], AF.Ln, ['ass'], ['arstd'], bias=EPS, scale=1.0 / 512)
            ACT(S, arstd[:], arstd[:], AF.Exp, ['arstd'], ['arstd'], scale=-0.5)
            ACT(S, att_b[ab][:], att_o[:], AF.Copy, ['att_o', 'arstd'], ['att_b%d' % ab], scale=arstd[:, 0:1])
            DMA(S, 'pool', mix_s[r0:r0 + 128, 0:512], att_b[ab][:], ['att_b%d' % ab], [], 'ao%d' % ab)

        load_I(0)
        for _ in I_block(0, 0):
            pass
        load_A(0)
        if NT > 1:
            load_I(1)
        B_stage(0)
        for i in range(NT):
            nkbA = (i + 4) // 4
            nkbI = (i + 5) // 4 if i + 1 < NT else 0
            for g in range(max(nkbA, nkbI)):
                gA = A_group(i, g) if g < nkbA else iter(())
                gI = I_block(i + 1, g) if g < nkbI else iter(())
                doneA = doneI = False
                while not (doneA and doneI):
                    if not doneA:
                        try:
                            next(gA)
                        except StopIteration:
                            doneA = True
                    for _ in range(2):
                        if not doneI:
                            try:
                                next(gI)
                            except StopIteration:
                                doneI = True
            A_finish(i)
            if i + 1 < NT:
                load_A(i + 1)
                if i + 2 < NT:
                    load_I(i + 2)
                B_stage(i + 1)
        S.emit("p2")


def phase3(nc, S, L, x, w_out, w_gate_up, w_down, mixg_pm, fing_bc, modT, AB, identb, identf, onesf,
           mix_s, out):
    NS3 = L // 256
    with ExitStack() as ph3:
        def sbw(name, shape, dt):
            return ph3.enter_context(nc.sbuf_tensor(name, list(shape), dt))

        wo = sbw("wo", [128, 8, D], BF16)
        wgu = sbw("wgu", [128, 8, 2 * DFF], BF16)
        wd = sbw("wd", [128, 22, D], BF16)
        g1_bc = sbw("g1_bc", [128, D], F32)
        g2_bc = sbw("g2_bc", [128, D], F32)
        fing = sbw("fing", [128, D], F32)
        mixg = sbw("mixg", [128, 8], F32)

        with ExitStack() as ph:
            def sbp(name, shape, dt):
                return ph.enter_context(nc.sbuf_tensor(name, list(shape), dt))

            def psp(name, shape, dt):
                return ph.enter_context(nc.psum_tensor(name, list(shape), dt))

            stg = [sbp("stg3_%d" % i, [128, 2 * DFF], F32) for i in range(2)]
            tmpf = sbp("tmpf", [128, 128], F32)
            pB = psp("pB", [128, 128], F32)
            si = [0]

            def load_cast(dst, src_rows, ncols, key):
                b = si[0] % 2
                si[0] += 1
                DMA(S, 'sp' if b == 0 else 'pool', stg[b][:, 0:ncols], src_rows, [], ['stg%d' % b],
                    'stg%d' % b)
                h1 = ncols // 2
                CP(S, 'dve', dst[:, 0:h1], stg[b][:, 0:h1], ['stg%d' % b], [key + 'a'])
                CP(S, 'act', dst[:, h1:ncols], stg[b][:, h1:ncols], ['stg%d' % b], [key + 'b'])

            for k in range(8):
                load_cast(wgu[:, k, :], w_gate_up[k * 128:(k + 1) * 128, :], 2 * DFF, 'wgu%d' % k)
            for k in range(8):
                load_cast(wo[:, k, :], w_out[k * 128:(k + 1) * 128, :], D, 'wo%d' % k)
            for k in range(22):
                load_cast(wd[:, k, :], w_down[k * 128:(k + 1) * 128, :], D, 'wd%d' % k)
            DMA(S, 'sp', fing[:], fing_bc[:, :], [], ['fing'], 'fing')
            DMA(S, 'sp', mixg[:], mixg_pm[:, :], [], ['mixg'], 'mixg')
            for (dst, c0, nm) in ((g1_bc, 16, 'g1_bc'), (g2_bc, 40, 'g2_bc')):
                for k in range(8):
                    TS(S, 'dve', tmpf[:], onesf[:], modT[:, c0 + k:c0 + k + 1], None, ALU.mult, None,
                       ['onesf', 'modT'], ['tmpf'])
                    MM(S, pB[:, :], tmpf[:], identf[:], True, True, ['tmpf', 'identf'], ['pB'])
                    CP(S, 'dve', dst[:, k * 128:(k + 1) * 128], pB[:, :], ['pB'], [nm])
            S.emit("p3a")

        with ExitStack() as ph:
            def sbp(name, shape, dt):
                return ph.enter_context(nc.sbuf_tensor(name, list(shape), dt))

            def psp(name, shape, dt):
                return ph.enter_context(nc.psum_tensor(name, list(shape), dt))

            x_sb = sbp("x3", [128, 2, D], F32)
            mixb = sbp("mixb", [128, 2, D], BF16)
            mixT = sbp("mixT", [128, 8, 256], BF16)
            h2T = sbp("h2T", [128, 8, 256], BF16)
            actT = sbp("actT", [128, 22, 256], BF16)
            tmp5 = [sbp("tmp5_%d" % i, [128, 512], F32) for i in range(2)]
            ss = sbp("ss3", [128, 2], F32)
            rstd = sbp("rstd3", [128, 2], F32)
            xn = [sbp("xn3_%d" % i, [128, D], BF16) for i in range(2)]
            eg = [sbp("eg%d" % i, [128, 256], F32) for i in range(2)]

            pT = [psp("p3T%d" % i, [128, D], BF16) for i in range(2)]
            pA = [psp("p3A%d" % i, [128, 512], F32) for i in range(2)]
            pG = [psp("p3G%d" % i, [128, 512], F32) for i in range(2)]
            pD = [psp("p3D%d" % i, [128, 512], F32) for i in range(2)]
            WO = ['wo%d%s' % (k, a) for k in range(8) for a in 'ab']
            WGU = ['wgu%d%s' % (k, a) for k in range(8) for a in 'ab']
            WD = ['wd%d%s' % (k, a) for k in range(22) for a in 'ab']
            ti = [0]
            gi = [0]

            def rms_stats(j, b):
                ACT(S, xn[b][:], x_sb[:, j, :], AF.Square, ['x3'], ['xn3_%d' % b, 'ss3'], accum=ss[:, j:j + 1])
                ACT(S, rstd[:, j:j + 1], ss[:, j:j + 1], AF.Ln, ['ss3'], ['rstd3'], bias=EPS, scale=1.0 / D)
                ACT(S, rstd[:, j:j + 1], rstd[:, j:j + 1], AF.Exp, ['rstd3'], ['rstd3'], scale=-0.5)

            for st in range(NS3):
                t0 = st * 256
                DMA(S, 'sp', x_sb[:], x[t0:t0 + 256, :].rearrange("(j p) d -> p j d", p=128), [], ['x3'], 'x3')
                DMA(S, 'sp', mixb[:], mix_s[t0:t0 + 256, :].rearrange("(j p) d -> p j d", p=128), [],
                    ['mixb'], 'mixb')
                for j in range(2):
                    b = ti[0] % 2
                    ti[0] += 1
                    for k in range(8):
                        TR(S, pT[b][:, k * 128:(k + 1) * 128], mixb[:, j, k * 128:(k + 1) * 128], identb[:],
                           ['mixb', 'identb'], ['p3T%d' % b])
                    for k in range(8):
                        TS(S, 'dve', mixT[:, k, j * 128:(j + 1) * 128], pT[b][:, k * 128:(k + 1) * 128],
                           mixg[:, k:k + 1], None, ALU.mult, None, ['p3T%d' % b, 'mixg'], ['mixT'])
                for j in range(2):
                    for nb in range(2):
                        cs = slice(nb * 512, (nb + 1) * 512)
                        for k in range(8):
                            MM(S, pA[nb][:, :], mixT[:, k, j * 128:(j + 1) * 128], wo[:, k, cs], k == 0, k == 7,
                               ['mixT'] + WO, ['p3A%d' % nb])
                        TT(S, 'dve', tmp5[nb][:], pA[nb][:, :], g1_bc[:, cs], ALU.mult,
                           ['p3A%d' % nb, 'g1_bc'], ['tmp5_%d' % nb])
                        TT(S, 'pool', x_sb[:, j, cs], x_sb[:, j, cs], tmp5[nb][:], ALU.add,
                           ['x3', 'tmp5_%d' % nb], ['x3'])
                for j in range(2):
                    b = ti[0] % 2
                    ti[0] += 1
                    rms_stats(j, b)
                    ACT(S, xn[b][:], x_sb[:, j, :], AF.Copy, ['x3', 'rstd3'], ['xn3_%d' % b],
                        scale=rstd[:, j:j + 1])
                    for k in range(8):
                        TR(S, pT[b][:, k * 128:(k + 1) * 128], xn[b][:, k * 128:(k + 1) * 128], identb[:],
                           ['xn3_%d' % b, 'identb'], ['p3T%d' % b])
                    for k in range(8):
                        TS(S, 'dve', h2T[:, k, j * 128:(j + 1) * 128], pT[b][:, k * 128:(k + 1) * 128],
                           AB[:, 16 + k:17 + k], AB[:, 24 + k:25 + k], ALU.mult, ALU.add,
                           ['p3T%d' % b, 'AB2', 'AB3'], ['h2T'])
                for f in range(22):
                    g = gi[0] % 2
                    gi[0] += 1
                    for k in range(8):
                        MM(S, pG[g][:, 0:256], wgu[:, k, f * 128:(f + 1) * 128], h2T[:, k, :], k == 0, k == 7,
                           ['h2T'] + WGU, ['p3G%d' % g])
                    for k in range(8):
                        MM(S, pG[g][:, 256:512], wgu[:, k, DFF + f * 128:DFF + (f + 1) * 128], h2T[:, k, :],
                           k == 0, k == 7, ['h2T'] + WGU, ['p3G%d' % g])
                    ACT(S, eg[g][:], pG[g][:, 0:256], AF.Silu, ['p3G%d' % g], ['eg%d' % g])
                    TT(S, 'dve', actT[:, f, :], pG[g][:, 256:512], eg[g][:], ALU.mult,
                       ['p3G%d' % g, 'eg%d' % g], ['actT'])
                for j in range(2):
                    for nb in range(2):
                        cs = slice(nb * 512, (nb + 1) * 512)
                        for f in range(22):
                            MM(S, pD[nb][:, :], actT[:, f, j * 128:(j + 1) * 128], wd[:, f, cs], f == 0, f == 21,
                               ['actT'] + WD, ['p3D%d' % nb])
                        TT(S, 'dve', tmp5[nb][:], pD[nb][:, :], g2_bc[:, cs], ALU.mult,
                           ['p3D%d' % nb, 'g2_bc'], ['tmp5_%d' % nb])
                        TT(S, 'pool', x_sb[:, j, cs], x_sb[:, j, cs], tmp5[nb][:], ALU.add,
                           ['x3', 'tmp5_%d' % nb], ['x3'])
                    b = ti[0] % 2
                    ti[0] += 1
                    rms_stats(j, b)
                    ACT(S, x_sb[:, j, :], x_sb[:, j, :], AF.Copy, ['x3', 'rstd3'], ['x3'], scale=rstd[:, j:j + 1])
                    TT(S, 'pool', x_sb[:, j, :], x_sb[:, j, :], fing[:], ALU.mult, ['x3', 'fing'], ['x3'])
                    DMA(S, 'pool', out[t0 + j * 128:t0 + (j + 1) * 128, :], x_sb[:, j, :], ['x3'], [], 'yo%d' % j)
            S.emit("p3b")
```
